# Optimizing a Trainium2 kernel written in Bass

```python
import math
import jax, jax.numpy as jnp
from jax import lax
import numpy as np

D_MODEL = 1024
BATCH = 2
SEQ = 8192
DEPTH = 2

HEAD_DIM = 64
Q_BLOCK = 128
NORM_EPS = 1e-6

RWKV_HEADS = 8
RWKV_WIDTH = RWKV_HEADS * HEAD_DIM
RWKV_DECAY_RANK = 64
RWKV_ICLR_RANK = 64
RWKV_GATE_RANK = 128
RWKV_GN_EPS = 64e-5

NSA_Q_HEADS = 8
NSA_KV_GROUPS = 2
NSA_CMP_LEN = 32
NSA_CMP_STRIDE = 16
NSA_CMP_HIDDEN = 256
NSA_SEL_BLOCK = 64
NSA_SEL_TOPN = 16
NSA_WINDOW = 512
NSA_FORCE_SCORE = 1e9

SWA_Q_HEADS = 8
SWA_KV_HEADS = 2
SWA_WINDOW = 128

REL_BUCKETS = 32
REL_MAX_DIST = 128

PEER_HEADS = 8
PEER_NKEYS = 128
PEER_EXPERTS = PEER_NKEYS * PEER_NKEYS
PEER_QDIM = 256
PEER_TOPK = 16
PEER_TOK_BLOCK = 128

RWKV_SPLITS = (RWKV_WIDTH, RWKV_WIDTH, RWKV_WIDTH, RWKV_DECAY_RANK, RWKV_ICLR_RANK, RWKV_GATE_RANK)
NSA_SPLITS = (NSA_Q_HEADS * HEAD_DIM,) + (NSA_KV_GROUPS * HEAD_DIM,) * 6 + (NSA_Q_HEADS * 3,)
SWA_SPLITS = (SWA_Q_HEADS * HEAD_DIM, SWA_KV_HEADS * HEAD_DIM, SWA_KV_HEADS * HEAD_DIM)
GATE_SPLITS = (D_MODEL, D_MODEL, D_MODEL)
RWKV_COLS = sum(RWKV_SPLITS)
IN_SPLITS = RWKV_SPLITS + NSA_SPLITS + SWA_SPLITS + GATE_SPLITS
IN_COLS = sum(IN_SPLITS)

kernel_name = 'hybrid_rwkv7_nsa_swasink_peer'


def split_cols(z, sizes):
    cuts = [int(c) for c in np.cumsum(sizes)[:-1]]
    return jnp.split(z, cuts, axis=-1)


def rmsnorm(x, w):
    xf = x.astype(jnp.float32)
    y = xf * lax.rsqrt(jnp.mean(xf * xf, axis=-1, keepdims=True) + NORM_EPS)
    return (y * w.astype(jnp.float32)).astype(x.dtype)


def token_shift(z, mu):
    z_prev = jnp.pad(z, ((0, 0), (1, 0), (0, 0)))[:, :-1]
    return z + mu * (z_prev - z)


def t5_bucket(dist):
    n = jnp.maximum(dist, 0)
    max_exact = REL_BUCKETS // 2
    nf = jnp.maximum(n, max_exact).astype(jnp.float32)
    large = max_exact + (jnp.log(nf / max_exact) / math.log(REL_MAX_DIST / max_exact)
                         * (REL_BUCKETS - max_exact)).astype(jnp.int32)
    large = jnp.minimum(large, REL_BUCKETS - 1)
    return jnp.where(n < max_exact, n, large)


def masked_softmax(s, mask):
    s = jnp.where(mask, s.astype(jnp.float32), -jnp.inf)
    m = jnp.max(s, axis=-1, keepdims=True)
    m = jnp.where(jnp.isfinite(m), m, 0.0)
    e = jnp.exp(s - m)
    return e / jnp.maximum(jnp.sum(e, axis=-1, keepdims=True), 1e-30)


def sink_softmax(s, mask, sink):
    s = jnp.where(mask, s.astype(jnp.float32), -jnp.inf)
    sink = sink.astype(jnp.float32)[None, :, :, None, None]
    m = jnp.maximum(jnp.max(s, axis=-1, keepdims=True), sink)
    e = jnp.exp(s - m)
    return e / (jnp.sum(e, axis=-1, keepdims=True) + jnp.exp(sink - m))


def rwkv7_mixer(zr, zk, zv, zw, za, zg, w0, w2, a0, a2, g2, k_k, k_a, r_k, ln_w, ln_b):
    B, S, C = zr.shape
    H, N = RWKV_HEADS, HEAD_DIM
    f32 = jnp.float32
    log_w = -jax.nn.softplus(-(w0 + jnp.tanh(zw) @ w2)) - 0.5
    decay = jnp.exp(-jnp.exp(log_w.astype(f32)))
    a = jax.nn.sigmoid(a0 + za @ a2)
    g = jax.nn.sigmoid(zg) @ g2
    kk = (zk * k_k).reshape(B, S, H, N).astype(f32)
    kk = kk * lax.rsqrt(jnp.maximum(jnp.sum(kk * kk, axis=-1, keepdims=True), 1e-12))
    k = zk * (1.0 + (a - 1.0) * k_a)
    hd = lambda z: z.reshape(B, S, H, N).astype(f32)
    r_h, k_h, v_h, a_h, w_h = hd(zr), hd(k), hd(zv), hd(a), hd(decay)
    tm = lambda z: jnp.swapaxes(z, 0, 1)

    def step(state, inp):
        r_t, w_t, k_t, v_t, kk_t, a_t = inp
        sa = jnp.einsum('bhij,bhj->bhi', state, -kk_t)
        state = (state * w_t[:, :, None, :] + sa[..., None] * (kk_t * a_t)[:, :, None, :]
                 + v_t[..., None] * k_t[:, :, None, :])
        return state, jnp.einsum('bhij,bhj->bhi', state, r_t)

    s0 = jnp.zeros((B, H, N, N), f32)
    _, y = lax.scan(step, s0, (tm(r_h), tm(w_h), tm(k_h), tm(v_h), tm(kk), tm(a_h)))
    y = tm(y)
    mu = jnp.mean(y, axis=-1, keepdims=True)
    var = jnp.mean(jnp.square(y - mu), axis=-1, keepdims=True)
    y = ((y - mu) * lax.rsqrt(var + RWKV_GN_EPS)).reshape(B, S, C) * ln_w + ln_b
    bonus = jnp.sum(r_h * k_h * r_k, axis=-1, keepdims=True) * v_h
    out = (y + bonus.reshape(B, S, C)) * g
    return out.astype(zr.dtype)


def nsa_compress(z, pe, w1, w2):
    B, S, G, dh = z.shape
    n_sub = NSA_CMP_LEN // NSA_CMP_STRIDE
    chunks = z.reshape(B, S // NSA_CMP_STRIDE, NSA_CMP_STRIDE, G, dh)
    n_cmp = S // NSA_CMP_STRIDE - n_sub + 1
    blocks = jnp.concatenate([chunks[:, j:j + n_cmp] for j in range(n_sub)], axis=2)
    blocks = blocks + pe[None, None, :, None, :]
    flat = blocks.transpose(0, 3, 1, 2, 4).reshape(B, G, n_cmp, NSA_CMP_LEN * dh)
    return jax.nn.gelu(flat @ w1, approximate=False) @ w2


def nsa_mixer(q, kc, vc, ks, vs, kw, vw, gates, pe_k, pe_v, ck_w1, ck_w2, cv_w1, cv_w2, bias_tbl):
    B, S, _ = q.shape
    G, Hg, dh = NSA_KV_GROUPS, NSA_Q_HEADS // NSA_KV_GROUPS, HEAD_DIM
    scale = dh ** -0.5
    heads = lambda z: z.reshape(B, S, G, dh)
    kv_t = lambda z: heads(z).transpose(0, 2, 1, 3)
    qh = q.reshape(B, S, G, Hg, dh).transpose(0, 2, 3, 1, 4)
    gh = jax.nn.sigmoid(gates.reshape(B, S, G, Hg, 3)).transpose(0, 2, 3, 1, 4)

    k_cmp = nsa_compress(heads(kc), pe_k, ck_w1, ck_w2)
    v_cmp = nsa_compress(heads(vc), pe_v, cv_w1, cv_w2)
    n_cmp = k_cmp.shape[2]
    cmp_start = jnp.arange(n_cmp) * NSA_CMP_STRIDE
    cmp_end = cmp_start + NSA_CMP_LEN - 1
    n_sel = S // NSA_SEL_BLOCK
    sel_start = jnp.arange(n_sel) * NSA_SEL_BLOCK
    overlap = ((cmp_end[:, None] >= sel_start[None, :])
               & (cmp_start[:, None] <= sel_start[None, :] + NSA_SEL_BLOCK - 1)).astype(jnp.float32)
    ks_blk = kv_t(ks).reshape(B, G, n_sel, NSA_SEL_BLOCK, dh)
    vs_blk = kv_t(vs).reshape(B, G, n_sel, NSA_SEL_BLOCK, dh)
    pad = ((0, 0), (0, 0), (NSA_WINDOW, 0), (0, 0))
    kw_pad = jnp.pad(kv_t(kw), pad)
    vw_pad = jnp.pad(kv_t(vw), pad)

    tbl = bias_tbl.T.reshape(G, Hg, REL_BUCKETS)
    win_len = NSA_WINDOW + Q_BLOCK
    rel = jnp.arange(Q_BLOCK)[:, None] + NSA_WINDOW - jnp.arange(win_len)[None, :]
    win_bias = tbl[:, :, t5_bucket(rel)]
    win_rel_mask = (rel >= 0) & (rel < NSA_WINDOW)
    top_n = min(NSA_SEL_TOPN, n_sel)
    b_ix = jnp.arange(B)[:, None, None, None]
    g_ix = jnp.arange(G)[None, :, None, None]
    g_ix5 = jnp.arange(G)[None, :, None, None, None]
    h_ix5 = jnp.arange(Hg)[None, None, :, None, None]
    blk = jnp.arange(n_sel)

    def block(i):
        s0 = i * Q_BLOCK
        t = s0 + jnp.arange(Q_BLOCK)
        qb = lax.dynamic_slice_in_dim(qh, s0, Q_BLOCK, axis=3)
        gb = lax.dynamic_slice_in_dim(gh, s0, Q_BLOCK, axis=3)
        sc = jnp.einsum('bghqd,bgcd->bghqc', qb, k_cmp) * scale
        pc = masked_softmax(sc, cmp_end[None, :] <= t[:, None])
        o_cmp = jnp.einsum('bghqc,bgcd->bghqd', pc.astype(v_cmp.dtype), v_cmp)
        imp = jnp.einsum('bghqc,cn->bgqn', pc, overlap)
        cur = t // NSA_SEL_BLOCK
        forced = (blk[None, :] == 0) | (blk[None, :] == cur[:, None]) | (blk[None, :] == cur[:, None] - 1)
        valid = blk[None, :] <= cur[:, None]
        imp = jnp.where(forced, NSA_FORCE_SCORE, jnp.where(valid, imp, -1.0))
        _, sel = lax.top_k(imp, top_n)
        k_sel = ks_blk[b_ix, g_ix, sel].reshape(B, G, Q_BLOCK, top_n * NSA_SEL_BLOCK, dh)
        v_sel = vs_blk[b_ix, g_ix, sel].reshape(B, G, Q_BLOCK, top_n * NSA_SEL_BLOCK, dh)
        pos = (sel[..., None] * NSA_SEL_BLOCK + jnp.arange(NSA_SEL_BLOCK)).reshape(B, G, Q_BLOCK, top_n * NSA_SEL_BLOCK)
        dist = t[:, None] - pos
        bias = tbl[g_ix5, h_ix5, t5_bucket(dist)[:, :, None]]
        ss = jnp.einsum('bghqd,bgqkd->bghqk', qb, k_sel) * scale + bias
        ps = masked_softmax(ss, (dist >= 0)[:, :, None])
        o_sel = jnp.einsum('bghqk,bgqkd->bghqd', ps.astype(v_sel.dtype), v_sel)
        k_win = lax.dynamic_slice_in_dim(kw_pad, s0, win_len, axis=2)
        v_win = lax.dynamic_slice_in_dim(vw_pad, s0, win_len, axis=2)
        kpos = s0 - NSA_WINDOW + jnp.arange(win_len)
        sw = jnp.einsum('bghqd,bgkd->bghqk', qb, k_win) * scale + win_bias
        pw = masked_softmax(sw, win_rel_mask & (kpos >= 0)[None, :])
        o_win = jnp.einsum('bghqk,bgkd->bghqd', pw.astype(v_win.dtype), v_win)
        return gb[..., 0:1] * o_cmp + gb[..., 1:2] * o_sel + gb[..., 2:3] * o_win

    out = lax.map(block, jnp.arange(S // Q_BLOCK))
    return out.transpose(1, 0, 4, 2, 3, 5).reshape(B, S, G * Hg * dh)


def swa_sink_mixer(q, k, v, sinks, bias_tbl):
    B, S, _ = q.shape
    G, Hg, dh = SWA_KV_HEADS, SWA_Q_HEADS // SWA_KV_HEADS, HEAD_DIM
    scale = dh ** -0.5
    qh = q.reshape(B, S, G, Hg, dh).transpose(0, 2, 3, 1, 4)
    pad = ((0, 0), (0, 0), (SWA_WINDOW, 0), (0, 0))
    k_pad = jnp.pad(k.reshape(B, S, G, dh).transpose(0, 2, 1, 3), pad)
    v_pad = jnp.pad(v.reshape(B, S, G, dh).transpose(0, 2, 1, 3), pad)
    win_len = SWA_WINDOW + Q_BLOCK
    rel = jnp.arange(Q_BLOCK)[:, None] + SWA_WINDOW - jnp.arange(win_len)[None, :]
    bias = bias_tbl.T.reshape(G, Hg, REL_BUCKETS)[:, :, t5_bucket(rel)]
    rel_mask = (rel >= 0) & (rel < SWA_WINDOW)
    sink = sinks.reshape(G, Hg)

    def block(i):
        s0 = i * Q_BLOCK
        qb = lax.dynamic_slice_in_dim(qh, s0, Q_BLOCK, axis=3)
        kb = lax.dynamic_slice_in_dim(k_pad, s0, win_len, axis=2)
        vb = lax.dynamic_slice_in_dim(v_pad, s0, win_len, axis=2)
        kpos = s0 - SWA_WINDOW + jnp.arange(win_len)
        s = jnp.einsum('bghqd,bgkd->bghqk', qb, kb) * scale + bias
        p = sink_softmax(s, rel_mask & (kpos >= 0)[None, :], sink)
        return jnp.einsum('bghqk,bgkd->bghqd', p.astype(vb.dtype), vb)

    out = lax.map(block, jnp.arange(S // Q_BLOCK))
    return out.transpose(1, 0, 4, 2, 3, 5).reshape(B, S, G * Hg * dh)


def peer_ffn(z, wq, subkeys, u_tab, v_tab):
    B, S, D = z.shape
    Hp, K, half = PEER_HEADS, PEER_TOPK, PEER_QDIM // 2
    zt = z.reshape(B * S // PEER_TOK_BLOCK, PEER_TOK_BLOCK, D)

    def block(zb):
        q = (zb @ wq).reshape(PEER_TOK_BLOCK, Hp, 2, half)
        s = jnp.einsum('thpd,hpnd->thpn', q, subkeys).astype(jnp.float32)
        s_top, i_top = lax.top_k(s, K)
        cand = (s_top[:, :, 0, :, None] + s_top[:, :, 1, None, :]).reshape(PEER_TOK_BLOCK, Hp, K * K)
        score, flat = lax.top_k(cand, K)
        i1 = jnp.take_along_axis(i_top[:, :, 0], flat // K, axis=-1)
        i2 = jnp.take_along_axis(i_top[:, :, 1], flat % K, axis=-1)
        expert = i1 * PEER_NKEYS + i2
        gate = jax.nn.softmax(score, axis=-1)
        h = jnp.einsum('thkd,td->thk', u_tab[expert], zb).astype(jnp.float32)
        act = (gate * jax.nn.gelu(h, approximate=False)).astype(zb.dtype)
        return jnp.einsum('thk,thkd->td', act, v_tab[expert])

    return lax.map(block, zt).reshape(B, S, D)


def setup_inputs(seed: int = 0) -> dict:
    key = jax.random.key(seed)
    keys = iter(jax.random.split(key, 40))
    L, D = DEPTH, D_MODEL

    def nrm(shape, scale):
        return jax.random.normal(next(keys), shape, jnp.float32) * scale

    def unif(shape, lo, hi):
        return jax.random.uniform(next(keys), shape, jnp.float32, lo, hi)

    cmp_in = NSA_CMP_LEN * HEAD_DIM
    return {
        'x': nrm((BATCH, SEQ, D), 1.0),
        'ln1_w': 1.0 + nrm((L, D), 0.02),
        'ln2_w': 1.0 + nrm((L, D), 0.02),
        'lnf_w': 1.0 + nrm((D,), 0.02),
        'rel_bias': nrm((REL_BUCKETS, NSA_Q_HEADS + SWA_Q_HEADS), 0.3),
        'w_in': nrm((L, D, IN_COLS), D ** -0.5),
        'rwkv_mu': unif((L, RWKV_COLS), 0.0, 1.0),
        'rwkv_w0': -0.5 + nrm((L, RWKV_WIDTH), 0.5),
        'rwkv_w2': nrm((L, RWKV_DECAY_RANK, RWKV_WIDTH), 0.1),
        'rwkv_a0': nrm((L, RWKV_WIDTH), 0.3),
        'rwkv_a2': nrm((L, RWKV_ICLR_RANK, RWKV_WIDTH), 0.1),
        'rwkv_g2': nrm((L, RWKV_GATE_RANK, RWKV_WIDTH), RWKV_GATE_RANK ** -0.5),
        'rwkv_k_k': 0.85 + nrm((L, RWKV_WIDTH), 0.05),
        'rwkv_k_a': 1.0 + nrm((L, RWKV_WIDTH), 0.05),
        'rwkv_r_k': nrm((L, RWKV_HEADS, HEAD_DIM), 0.1),
        'rwkv_ln_w': 1.0 + nrm((L, RWKV_WIDTH), 0.02),
        'rwkv_ln_b': nrm((L, RWKV_WIDTH), 0.02),
        'nsa_pe_k': nrm((L, NSA_CMP_LEN, HEAD_DIM), 0.1),
        'nsa_pe_v': nrm((L, NSA_CMP_LEN, HEAD_DIM), 0.1),
        'nsa_ck_w1': nrm((L, cmp_in, NSA_CMP_HIDDEN), cmp_in ** -0.5),
        'nsa_ck_w2': nrm((L, NSA_CMP_HIDDEN, HEAD_DIM), NSA_CMP_HIDDEN ** -0.5),
        'nsa_cv_w1': nrm((L, cmp_in, NSA_CMP_HIDDEN), cmp_in ** -0.5),
        'nsa_cv_w2': nrm((L, NSA_CMP_HIDDEN, HEAD_DIM), NSA_CMP_HIDDEN ** -0.5),
        'swa_sinks': nrm((L, SWA_Q_HEADS), 0.5),
        'w_br_a': nrm((L, RWKV_WIDTH, D), RWKV_WIDTH ** -0.5),
        'w_br_b': nrm((L, NSA_Q_HEADS * HEAD_DIM, D), (NSA_Q_HEADS * HEAD_DIM) ** -0.5),
        'w_br_c': nrm((L, SWA_Q_HEADS * HEAD_DIM, D), (SWA_Q_HEADS * HEAD_DIM) ** -0.5),
        'w_out': nrm((L, D, D), D ** -0.5),
        'peer_wq': nrm((L, D, PEER_HEADS * PEER_QDIM), D ** -0.5),
        'peer_subkeys': nrm((L, PEER_HEADS, 2, PEER_NKEYS, PEER_QDIM // 2), (PEER_QDIM // 2) ** -0.5),
        'peer_u': nrm((L, PEER_EXPERTS, D), D ** -0.5),
        'peer_v': nrm((L, PEER_EXPERTS, D), 0.5 * PEER_HEADS ** -0.5),
    }


def reference(x, ln1_w, ln2_w, lnf_w, rel_bias, w_in, rwkv_mu, rwkv_w0, rwkv_w2, rwkv_a0, rwkv_a2,
              rwkv_g2, rwkv_k_k, rwkv_k_a, rwkv_r_k, rwkv_ln_w, rwkv_ln_b, nsa_pe_k, nsa_pe_v,
              nsa_ck_w1, nsa_ck_w2, nsa_cv_w1, nsa_cv_w2, swa_sinks, w_br_a, w_br_b, w_br_c, w_out,
              peer_wq, peer_subkeys, peer_u, peer_v):
    nsa_tbl = rel_bias[:, :NSA_Q_HEADS]
    swa_tbl = rel_bias[:, NSA_Q_HEADS:]
    for l in range(DEPTH):
        u = rmsnorm(x, ln1_w[l])
        proj = u @ w_in[l]
        rw = token_shift(proj[..., :RWKV_COLS], rwkv_mu[l])
        zr, zk, zv, zw, za, zg = split_cols(rw, RWKV_SPLITS)
        cols = split_cols(proj[..., RWKV_COLS:], NSA_SPLITS + SWA_SPLITS + GATE_SPLITS)
        nq, nkc, nvc, nks, nvs, nkw, nvw, ngate = cols[:8]
        sq, sk, sv = cols[8:11]
        g_a, g_b, g_c = [jax.nn.sigmoid(c) for c in cols[11:14]]
        o_a = rwkv7_mixer(zr, zk, zv, zw, za, zg, rwkv_w0[l], rwkv_w2[l], rwkv_a0[l], rwkv_a2[l],
                          rwkv_g2[l], rwkv_k_k[l], rwkv_k_a[l], rwkv_r_k[l], rwkv_ln_w[l], rwkv_ln_b[l])
        o_b = nsa_mixer(nq, nkc, nvc, nks, nvs, nkw, nvw, ngate, nsa_pe_k[l], nsa_pe_v[l],
                        nsa_ck_w1[l], nsa_ck_w2[l], nsa_cv_w1[l], nsa_cv_w2[l], nsa_tbl)
        o_c = swa_sink_mixer(sq, sk, sv, swa_sinks[l], swa_tbl)
        merged = g_a * (o_a @ w_br_a[l]) + g_b * (o_b @ w_br_b[l]) + g_c * (o_c @ w_br_c[l])
        x = x + merged @ w_out[l]
        x = x + peer_ffn(rmsnorm(x, ln2_w[l]), peer_wq[l], peer_subkeys[l], peer_u[l], peer_v[l])
    return rmsnorm(x, lnf_w)
```

```python
import sys, time
from contextlib import ExitStack
import numpy as np
import concourse.bass as bass
import concourse.mybir as mybir
from concourse.bass_utils import run_bass_kernel_spmd

F32 = mybir.dt.float32
BF16 = mybir.dt.bfloat16
I32 = mybir.dt.int32
U32 = mybir.dt.uint32
AF = mybir.ActivationFunctionType
ALU = mybir.AluOpType
AX = mybir.AxisListType

ENGS = ["pe", "dve", "act", "pool", "sp"]
EPOCH = 20000
NSLOT = 6


class Buf:
    __slots__ = ("w", "r", "name")

    def __init__(self, name=""):
        self.w = None
        self.r = {}
        self.name = name


class KB:
    def __init__(self, nc, stack):
        self.nc = nc
        self.stack = stack
        self.sem_stack = stack
        self.prog = {e: [] for e in ENGS}
        self.cnt = {}
        self.seen = {e: {} for e in ENGS}
        self.sems = {}
        self.epoch = {e: 0 for e in ENGS}
        self.slot_i = {e: 0 for e in ENGS}
        self.slot_epoch = {}
        self.eng_obj = {"pe": nc.tensor, "dve": nc.vector, "act": nc.scalar, "pool": nc.gpsimd, "sp": nc.sync}
        self.nops = 0

    def _sem(self, key):
        if key not in self.sems:
            self.sems[key] = self.sem_stack.enter_context(self.nc.semaphore("s_" + "_".join(str(k) for k in key)))
            self.cnt[key] = 0
        return self.sems[key]

    def _ckey(self, eng):
        key = ("c", eng, self.epoch[eng])
        self._sem(key)
        if self.cnt[key] >= EPOCH:
            self.epoch[eng] += 1
            key = ("c", eng, self.epoch[eng])
            self._sem(key)
        return key

    def _dkey(self, eng):
        s = self.slot_i[eng] % NSLOT
        self.slot_i[eng] += 1
        ep = self.slot_epoch.get((eng, s), 0)
        key = ("d", eng, s, ep)
        self._sem(key)
        prev = (key, self.cnt[key])
        if self.cnt[key] >= EPOCH:
            ep += 1
            self.slot_epoch[(eng, s)] = ep
            key = ("d", eng, s, ep)
            self._sem(key)
        return key, prev

    def _need(self, eng, needs, tok):
        if tok is None:
            return
        key, val = tok
        if self.seen[eng].get(key, 0) >= val:
            return
        if needs.get(key, 0) < val:
            needs[key] = val

    def _flush_waits(self, eng, needs):
        for key, val in needs.items():
            if self.seen[eng].get(key, 0) >= val:
                continue
            self.prog[eng].append(("wait", key, val))
            self.seen[eng][key] = val

    def op(self, eng, fns, reads=(), writes=(), dma=False):
        if not isinstance(fns, (list, tuple)):
            fns = [fns]
        needs = {}
        for b in reads:
            self._need(eng, needs, b.w)
        for b in writes:
            self._need(eng, needs, b.w)
            for k, v in b.r.items():
                self._need(eng, needs, (k, v))
        if dma:
            key, prev = self._dkey(eng)
            self._need(eng, needs, prev)
            inc = 16
        else:
            key = self._ckey(eng)
            inc = 1
        self._flush_waits(eng, needs)
        self.cnt[key] += inc
        tok = (key, self.cnt[key])
        self.prog[eng].append(("op", list(fns), key, inc))
        for b in reads:
            if b.r.get(key, 0) < tok[1]:
                b.r[key] = tok[1]
        for b in writes:
            b.w = tok
            b.r = {}
        self.nops += len(fns)
        return tok

    def finish(self, eng="sp"):
        needs = {}
        for key, val in self.cnt.items():
            if val > 0:
                self._need(eng, needs, (key, val))
        self._flush_waits(eng, needs)

    def emit(self):
        nc = self.nc
        with nc.Block() as block:
            def run(eng_name):
                def body(e):
                    for item in self.prog[eng_name]:
                        if item[0] == "wait":
                            e.wait_ge(self.sems[item[1]], item[2])
                        else:
                            _, fns, key, inc = item
                            ins = None
                            for f in fns:
                                ins = f(e)
                            ins.then_inc(self.sems[key], inc)
                return body
            if self.prog["sp"]:
                block.sync(run("sp"))
            if self.prog["pe"]:
                block.tensor(run("pe"))
            if self.prog["dve"]:
                block.vector(run("dve"))
            if self.prog["act"]:
                block.scalar(run("act"))
            if self.prog["pool"]:
                block.gpsimd(run("pool"))

    def sb(self, name, shape, dtype):
        return self.stack.enter_context(self.nc.sbuf_tensor(name, list(shape), dtype))

    def ps(self, name, shape, dtype=F32):
        return self.stack.enter_context(self.nc.psum_tensor(name, list(shape), dtype))


D = 1024
NEG = -30000.0


def t5_bucket_np(dist):
    import math
    n = np.maximum(dist, 0)
    max_exact = 16
    nf = np.maximum(n, max_exact).astype(np.float32)
    large = max_exact + (np.log(nf / max_exact) / math.log(128 / max_exact) * (32 - max_exact)).astype(np.int32)
    large = np.minimum(large, 31)
    return np.where(n < max_exact, n, large)


class NormProj:
    def __init__(self, kb, xT, lnw, S, eps=1e-6):
        self.kb = kb
        nc = kb.nc
        self.xT = xT
        self.S = S
        self.xv = xT.rearrange("(c p) t -> p c t", p=128)
        self.x_sb = [kb.sb(f"np_x{i}", [128, 8, 512], F32) for i in range(2)]
        self.x_b = [Buf() for _ in range(2)]
        self.sq_sb = kb.sb("np_sq", [128, 8, 512], BF16)
        self.sq_b = Buf()
        self.u_sb = [kb.sb(f"np_u{i}", [128, 8, 512], BF16) for i in range(2)]
        self.u_b = [Buf() for _ in range(2)]
        self.rstd_sb = kb.sb("np_rstd", [128, 512], F32)
        self.rstd_b = Buf()
        self.ssq_ps = kb.ps("np_ssq", [128, 512])
        self.ssq_b = Buf()
        self.ones = kb.sb("np_ones", [128, 128], BF16)
        self.ones_b = Buf()
        self.lnw_sb = kb.sb("np_lnw", [128, 8], F32)
        self.lnw_b = Buf()
        self.lnw32 = kb.sb("np_lnw32", [128, 8], F32)
        self.lnw32_b = Buf()
        self.eps = eps
        kb.op("dve", lambda e: e.memset(self.ones[:], 1.0), writes=[self.ones_b])
        self.epsc = kb.sb("np_epsc", [128, 1], F32); self.epsc_b = Buf()
        kb.op("dve", lambda e: e.memset(self.epsc[:], float(D * eps)), writes=[self.epsc_b])
        kb.op("sp", lambda e: e.dma_start(out=self.lnw_sb[:], in_=lnw), writes=[self.lnw_b], dma=True)
        kb.op("dve", lambda e: e.tensor_scalar(out=self.lnw32[:], in0=self.lnw_sb[:], scalar1=float(np.sqrt(D)),
                                               scalar2=None, op0=ALU.mult),
              reads=[self.lnw_b], writes=[self.lnw32_b])
        self.nt = S // 512

    def load(self, tt):
        kb = self.kb
        i = tt % 2
        kb.op("sp", lambda e, i=i, tt=tt: e.dma_start(out=self.x_sb[i][:], in_=self.xv[:, :, tt * 512:(tt + 1) * 512]),
              writes=[self.x_b[i]], dma=True)

    def norm(self, tt):
        self.stats(tt)
        kb = self.kb
        i = tt % 2
        x = self.x_sb[i]
        u = self.u_sb[i]
        for c in range(8):
            kb.op("dve", lambda e, c=c, x=x, u=u: e.scalar_tensor_tensor(out=u[:, c, :], in0=x[:, c, :], scalar=self.lnw32[:, c:c + 1],
                                                                      in1=self.rstd_sb[:], op0=ALU.mult, op1=ALU.mult),
                  reads=[self.x_b[i], self.rstd_b, self.lnw32_b], writes=[self.u_b[i]] if c == 7 else [])
        return u, self.u_b[i]

    def stats(self, tt):
        kb = self.kb
        i = tt % 2
        x = self.x_sb[i]
        kb.op("act", lambda e, x=x: e.activation(out=self.sq_sb[:], in_=x[:], func=AF.Square),
              reads=[self.x_b[i]], writes=[self.sq_b])
        kb.op("pe", [lambda e, c=c: e.matmul(self.ssq_ps[:], lhsT=self.ones[:], rhs=self.sq_sb[:, c, :],
                                             start=(c == 0), stop=(c == 7)) for c in range(8)],
              reads=[self.sq_b, self.ones_b], writes=[self.ssq_b])
        kb.op("act", lambda e: e.activation(out=self.rstd_sb[:], in_=self.ssq_ps[:], func=AF.Sqrt, bias=self.epsc[:, 0:1], scale=1.0),
              reads=[self.ssq_b, self.epsc_b], writes=[self.rstd_b])
        kb.op("dve", lambda e: e.reciprocal(out=self.rstd_sb[:], in_=self.rstd_sb[:]),
              reads=[self.rstd_b], writes=[self.rstd_b])


def build_swa(S):
    nc = bass.Bass("TRN2", target_bir_lowering=False)
    xT = nc.dram_tensor("xT", [D, S], F32, kind="ExternalInput").ap()
    lnw = nc.dram_tensor("lnw", [128, 8], F32, kind="ExternalInput").ap()
    w = nc.dram_tensor("w", [128, 8, 320], F32, kind="ExternalInput").ap()
    bias = nc.dram_tensor("bias", [128, 2, 256], F32, kind="ExternalInput").ap()
    sink = nc.dram_tensor("sink", [128, 2], F32, kind="ExternalInput").ap()
    ident = nc.dram_tensor("ident", [128, 128], F32, kind="ExternalInput").ap()
    out = nc.dram_tensor("out", [S, 128], F32, kind="ExternalOutput").ap()
    with ExitStack() as stack:
        kb = KB(nc, stack)
        npj = NormProj(kb, xT, lnw, S)
        w_f = kb.sb("w_f", [128, 8, 320], F32); w_fb = Buf()
        w_b = kb.sb("w_b", [128, 8, 320], BF16); w_bb = Buf()
        bias_sb = kb.sb("bias_s", [128, 2, 256], F32); bias_b = Buf()
        sink_sb = kb.sb("sink_s", [128, 2], F32); sink_b = Buf()
        id_f = kb.sb("id_f", [128, 128], F32); id_fb = Buf()
        id_b = kb.sb("id_b", [128, 128], BF16); id_bb = Buf()
        kb.op("sp", lambda e: e.dma_start(out=w_f[:], in_=w), writes=[w_fb], dma=True)
        kb.op("sp", lambda e: e.dma_start(out=bias_sb[:], in_=bias), writes=[bias_b], dma=True)
        kb.op("sp", lambda e: e.dma_start(out=sink_sb[:], in_=sink), writes=[sink_b], dma=True)
        kb.op("sp", lambda e: e.dma_start(out=id_f[:], in_=ident), writes=[id_fb], dma=True)
        kb.op("dve", lambda e: e.tensor_copy(out=w_b[:], in_=w_f[:]), reads=[w_fb], writes=[w_bb])
        kb.op("dve", lambda e: e.tensor_copy(out=id_b[:], in_=id_f[:]), reads=[id_fb], writes=[id_bb])

        nblk = S // 128
        qT = [kb.sb(f"qT{i}", [128, 512], BF16) for i in range(2)]; qT_b = [Buf() for _ in range(2)]
        kT = kb.sb("kT", [128, S + 128], BF16)
        kT_b = [Buf() for _ in range(nblk // 4)]
        v_sb = kb.sb("v", [128, nblk, 64], BF16); v_b = [Buf() for _ in range(nblk)]
        pq = [kb.ps(f"pq{i}", [128, 512]) for i in range(2)]; pq_b = [Buf() for _ in range(2)]
        pv = kb.ps("pv", [128, 512]); pv_b = Buf()
        psc = [kb.ps(f"psc{i}", [128, 512]) for i in range(2)]; psc_b = [Buf() for _ in range(2)]
        pet = kb.ps("pet", [128, 512], BF16); pet_b = Buf()
        po = kb.ps("po", [128, 512]); po_b = Buf()
        s_sb = [kb.sb(f"s{i}", [128, 256], F32) for i in range(2)]; s_b = [Buf() for _ in range(2)]
        e_sb = [kb.sb(f"e{i}", [128, 256], BF16) for i in range(2)]; e_b = [Buf() for _ in range(2)]
        eT_sb = [kb.sb(f"eT{i}", [128, 256], BF16) for i in range(2)]; eT_b = [Buf() for _ in range(2)]
        st = [kb.sb(f"st{i}", [128, 8], F32) for i in range(2)]; st_b = [Buf() for _ in range(2)]
        o_sb = [kb.sb(f"o{i}", [128, 128], F32) for i in range(2)]; o_b = [Buf() for _ in range(2)]

        npj.load(0)
        hc = 0
        for tt in range(npj.nt):
            if tt + 1 < npj.nt:
                npj.load(tt + 1)
            u, u_b = npj.norm(tt)
            for j in range(2):
                kb.op("pe", [lambda e, c=c, j=j, u=u: e.matmul(pq[j][:], lhsT=w_b[:, c, j * 128:(j + 1) * 128], rhs=u[:, c, :],
                                                         start=(c == 0), stop=(c == 7)) for c in range(8)],
                      reads=[u_b, w_bb], writes=[pq_b[j]])
            kb.op("act", lambda e, tt=tt: e.activation(out=qT[tt % 2][:], in_=pq[0][:], func=AF.Copy),
                  reads=[pq_b[0]], writes=[qT_b[tt % 2]])
            kb.op("act", lambda e, tt=tt: e.activation(out=kT[:, 128 + tt * 512:128 + (tt + 1) * 512], in_=pq[1][:], func=AF.Copy),
                  reads=[pq_b[1]], writes=[kT_b[tt]])
            for sbk in range(4):
                blk = tt * 4 + sbk
                kb.op("pe", [lambda e, c=c, sbk=sbk, u=u: e.matmul(pv[:, sbk * 64:(sbk + 1) * 64], lhsT=u[:, c, sbk * 128:(sbk + 1) * 128],
                                                             rhs=w_b[:, c, 256:320], start=(c == 0), stop=(c == 7)) for c in range(8)],
                      reads=[u_b, w_bb], writes=[pv_b])
                kb.op("dve", lambda e, sbk=sbk, blk=blk: e.tensor_copy(out=v_sb[:, blk, :], in_=pv[:, sbk * 64:(sbk + 1) * 64]),
                      reads=[pv_b], writes=[v_b[blk]])
            for sbk in range(4):
                blk = tt * 4 + sbk
                ob = blk % 2
                for h in range(2):
                    p = hc % 2
                    hc += 1
                    hs = slice(h * 64, (h + 1) * 64)
                    k0 = 0 if blk > 0 else 128
                    kc0 = blk * 128 + k0
                    nk = 256 - k0
                    rd_k = [kT_b[tt]] + ([kT_b[tt - 1]] if (sbk == 0 and tt > 0) else [])
                    kb.op("pe", lambda e, p=p, hs=hs, sbk=sbk, tt=tt, kc0=kc0, nk=nk: e.matmul(
                        psc[p][:, 0:nk], lhsT=qT[tt % 2][hs, sbk * 128:(sbk + 1) * 128], rhs=kT[hs, kc0:kc0 + nk], start=True, stop=True),
                        reads=[qT_b[tt % 2]] + rd_k, writes=[psc_b[p]])
                    kb.op("dve", lambda e, p=p, h=h, k0=k0, nk=nk: e.scalar_tensor_tensor(
                        out=s_sb[p][:, 0:nk], in0=psc[p][:, 0:nk], scalar=0.125, in1=bias_sb[:, h, k0:256], op0=ALU.mult, op1=ALU.add),
                        reads=[psc_b[p], bias_b], writes=[s_b[p]])
                    kb.op("dve", lambda e, p=p, nk=nk: e.tensor_reduce(out=st[p][:, 0:1], in_=s_sb[p][:, 0:nk], axis=AX.X, op=ALU.max),
                          reads=[s_b[p]], writes=[st_b[p]])
                    kb.op("dve", lambda e, p=p, h=h: e.tensor_scalar(out=st[p][:, 1:2], in0=st[p][:, 0:1], scalar1=sink_sb[:, h:h + 1],
                                                                    scalar2=-1.0, op0=ALU.max, op1=ALU.mult),
                          reads=[st_b[p], sink_b], writes=[st_b[p]])
                    kb.op("act", lambda e, p=p, nk=nk: e.activation(out=e_sb[p][:, 0:nk], in_=s_sb[p][:, 0:nk], func=AF.Exp,
                                                                   bias=st[p][:, 1:2], scale=1.0, accum_out=st[p][:, 2:3]),
                          reads=[s_b[p], st_b[p]], writes=[e_b[p], st_b[p]])
                    kb.op("act", lambda e, p=p, h=h: e.activation(out=st[p][:, 3:4], in_=sink_sb[:, h:h + 1], func=AF.Exp,
                                                                 bias=st[p][:, 1:2], scale=1.0),
                          reads=[sink_b, st_b[p]], writes=[st_b[p]])
                    kb.op("dve", lambda e, p=p: e.tensor_tensor(out=st[p][:, 4:5], in0=st[p][:, 2:3], in1=st[p][:, 3:4], op=ALU.add),
                          reads=[st_b[p]], writes=[st_b[p]])
                    kb.op("dve", lambda e, p=p: e.reciprocal(out=st[p][:, 5:6], in_=st[p][:, 4:5]),
                          reads=[st_b[p]], writes=[st_b[p]])
                    nkb = nk // 128
                    kb.op("pe", [lambda e, p=p, j=j: e.transpose(out=pet[:, j * 128:(j + 1) * 128], in_=e_sb[p][:, j * 128:(j + 1) * 128],
                                                                identity=id_b[:]) for j in range(nkb)],
                          reads=[e_b[p], id_bb], writes=[pet_b])
                    kb.op("dve", lambda e, p=p, nk=nk: e.tensor_copy(out=eT_sb[p][:, 0:nk], in_=pet[:, 0:nk]),
                          reads=[pet_b], writes=[eT_b[p]])
                    vblks = [blk - 1, blk] if blk > 0 else [blk]
                    kb.op("pe", [lambda e, p=p, j=j, vb=vb, h=h, n=len(vblks): e.matmul(
                        po[:, h * 64:(h + 1) * 64], lhsT=eT_sb[p][:, j * 128:(j + 1) * 128], rhs=v_sb[:, vb, :],
                        start=(j == 0), stop=(j == n - 1)) for j, vb in enumerate(vblks)],
                        reads=[eT_b[p]] + [v_b[vb] for vb in vblks], writes=[po_b])
                    kb.op("dve", lambda e, p=p, h=h, ob=ob: e.tensor_scalar(out=o_sb[ob][:, h * 64:(h + 1) * 64], in0=po[:, h * 64:(h + 1) * 64],
                                                                           scalar1=st[p][:, 5:6], scalar2=None, op0=ALU.mult),
                          reads=[po_b, st_b[p]], writes=[o_b[ob]])
                kb.op("sp", lambda e, ob=ob, blk=blk: e.dma_start(out=out[blk * 128:(blk + 1) * 128, :], in_=o_sb[ob][:]),
                      reads=[o_b[ob]], dma=True)
        kb.finish("sp")
        kb.emit()
    return nc


def swa_host_inputs(inp, l, b, hg, S, x_l):
    base = 1792 + 1304
    kvh = hg // 2
    W = inp["w_in"][l]
    cols = np.concatenate([np.arange(base + hg * 128, base + (hg + 1) * 128),
                           np.arange(base + 512 + kvh * 64, base + 512 + (kvh + 1) * 64),
                           np.arange(base + 512 + kvh * 64, base + 512 + (kvh + 1) * 64),
                           np.arange(base + 640 + kvh * 64, base + 640 + (kvh + 1) * 64)])
    w = np.ascontiguousarray(W[:, cols].reshape(8, 128, 320).transpose(1, 0, 2))
    tbl = inp["rel_bias"][:, 8:]
    q = np.arange(128)[:, None]; kk = np.arange(256)[None, :]
    rel = q + 128 - kk
    valid = (rel >= 0) & (rel < 128)
    bias = np.empty((128, 2, 256), np.float32)
    for h in range(2):
        bias[:, h, :] = np.where(valid, tbl[t5_bucket_np(rel), hg * 2 + h], NEG)
    sink = np.ascontiguousarray(np.broadcast_to(inp["swa_sinks"][l][hg * 2:hg * 2 + 2][None, :], (128, 2))).astype(np.float32)
    return {
        "xT": np.ascontiguousarray(x_l[b, :S].T),
        "lnw": np.ascontiguousarray(inp["ln1_w"][l].reshape(8, 128).T),
        "w": w, "bias": bias, "sink": sink, "ident": np.eye(128, dtype=np.float32),
    }


GATE0 = 1792 + 1304 + 768


def build_merge(T):
    nc = bass.Bass("TRN2", target_bir_lowering=False)
    xT = nc.dram_tensor("xT", [D, T], F32, kind="ExternalInput").ap()
    lnw = nc.dram_tensor("lnw", [128, 8], F32, kind="ExternalInput").ap()
    wg = nc.dram_tensor("wg", [128, 8, 3072], F32, kind="ExternalInput").ap()
    wbr = nc.dram_tensor("wbr", [128, 3, 4, 1024], F32, kind="ExternalInput").ap()
    wo = nc.dram_tensor("wo", [128, 8, 1024], F32, kind="ExternalInput").ap()
    oT = nc.dram_tensor("oT", [3, 512, T], F32, kind="ExternalInput").ap()
    out = nc.dram_tensor("out", [D, T], F32, kind="ExternalOutput").ap()
    with ExitStack() as stack:
        kb = KB(nc, stack)
        npj = NormProj(kb, xT, lnw, T)
        stg = [kb.sb(f"stg{i}", [128, 2048], F32) for i in range(2)]; stg_b = [Buf() for _ in range(2)]
        wg_b = kb.sb("wg_b", [128, 8, 3072], BF16); wg_bb = Buf()
        wbr_b = kb.sb("wbr_b", [128, 3, 4, 1024], BF16); wbr_bb = Buf()
        wo_b = kb.sb("wo_b", [128, 8, 1024], BF16); wo_bb = Buf()
        pieces = []
        for c in range(8):
            for hlf in range(2):
                pieces.append((wg[:, c, hlf * 1536:(hlf + 1) * 1536], wg_b[:, c, hlf * 1536:(hlf + 1) * 1536], 1536, wg_bb))
        for j in range(3):
            for kc in range(0, 4, 2):
                pieces.append((wbr[:, j, kc:kc + 2, :], wbr_b[:, j, kc:kc + 2, :], 2048, wbr_bb))
        for c in range(0, 8, 2):
            pieces.append((wo[:, c:c + 2, :], wo_b[:, c:c + 2, :], 2048, wo_bb))
        for i, (src, dst, n, bb) in enumerate(pieces):
            s = i % 2
            if len(src.shape) == 3:
                sv = stg[s][:, 0:n].rearrange("p (a b) -> p a b", a=src.shape[1])
            else:
                sv = stg[s][:, 0:n]
            kb.op("sp", lambda e, sv=sv, src=src: e.dma_start(out=sv, in_=src), writes=[stg_b[s]], dma=True)
            kb.op("dve", lambda e, sv=sv, dst=dst: e.tensor_copy(out=dst, in_=sv), reads=[stg_b[s]], writes=[bb])

        o_b = kb.sb("o_b16", [128, 3, 4, 512], BF16); o_bb = Buf()
        mg = kb.sb("mg", [128, 8, 512], BF16); mg_b = Buf()
        pg = [kb.ps(f"pg{i}", [128, 512]) for i in range(3)]; pg_b = [Buf() for _ in range(3)]
        pb = [kb.ps(f"pb{i}", [128, 512]) for i in range(3)]; pb_b = [Buf() for _ in range(3)]
        py = kb.ps("py", [128, 512]); py_b = Buf()
        g_sb = [kb.sb(f"g{i}", [128, 512], F32) for i in range(3)]; g_b = [Buf() for _ in range(3)]
        t_sb = [kb.sb(f"t{i}", [128, 512], F32) for i in range(3)]; t_b = [Buf() for _ in range(3)]
        y_sb = [kb.sb(f"y{i}", [128, 512], F32) for i in range(2)]; y_b = [Buf() for _ in range(2)]
        oTv = oT.rearrange("j (c p) t -> p j c t", p=128)
        outv = out.rearrange("(c p) t -> p c t", p=128)

        npj.load(0)
        for tt in range(npj.nt):
            if tt + 1 < npj.nt:
                npj.load(tt + 1)
            for j in range(3):
                s_ = j % 2
                sv = stg[s_][:, :].rearrange("p (a b) -> p a b", a=4)
                kb.op("sp", lambda e, j=j, tt=tt, sv=sv: e.dma_start(out=sv, in_=oTv[:, j, :, tt * 512:(tt + 1) * 512]),
                      writes=[stg_b[s_]], dma=True)
                kb.op("dve", lambda e, j=j, sv=sv: e.tensor_copy(out=o_b[:, j], in_=sv), reads=[stg_b[s_]], writes=[o_bb])
            u, u_b = npj.norm(tt)
            x = npj.x_sb[tt % 2]; x_b = npj.x_b[tt % 2]
            for cc in range(8):
                for j in range(3):
                    kb.op("pe", [lambda e, c=c, j=j, cc=cc, u=u: e.matmul(pg[j][:], lhsT=wg_b[:, c, j * 1024 + cc * 128:j * 1024 + (cc + 1) * 128],
                                                                       rhs=u[:, c, :], start=(c == 0), stop=(c == 7)) for c in range(8)],
                          reads=[u_b, wg_bb], writes=[pg_b[j]])
                    kb.op("pe", [lambda e, c=c, j=j, cc=cc: e.matmul(pb[j][:], lhsT=wbr_b[:, j, c, cc * 128:(cc + 1) * 128],
                                                                  rhs=o_b[:, j, c, :], start=(c == 0), stop=(c == 3)) for c in range(4)],
                          reads=[o_bb, wbr_bb], writes=[pb_b[j]])
                    kb.op("act", lambda e, j=j: e.activation(out=g_sb[j][:], in_=pg[j][:], func=AF.Sigmoid), reads=[pg_b[j]], writes=[g_b[j]])
                    kb.op("dve", lambda e, j=j: e.tensor_tensor(out=t_sb[j][:], in0=g_sb[j][:], in1=pb[j][:], op=ALU.mult),
                          reads=[g_b[j], pb_b[j]], writes=[t_b[j]])
                kb.op("dve", lambda e: e.tensor_tensor(out=t_sb[0][:], in0=t_sb[0][:], in1=t_sb[1][:], op=ALU.add),
                      reads=[t_b[0], t_b[1]], writes=[t_b[0]])
                kb.op("dve", lambda e, cc=cc: e.tensor_tensor(out=mg[:, cc, :], in0=t_sb[0][:], in1=t_sb[2][:], op=ALU.add),
                      reads=[t_b[0], t_b[2]], writes=[mg_b])
            for cc in range(8):
                kb.op("pe", [lambda e, c=c, cc=cc: e.matmul(py[:], lhsT=wo_b[:, c, cc * 128:(cc + 1) * 128], rhs=mg[:, c, :],
                                                         start=(c == 0), stop=(c == 7)) for c in range(8)],
                      reads=[mg_b, wo_bb], writes=[py_b])
                yi = cc % 2
                kb.op("dve", lambda e, cc=cc, yi=yi, x=x: e.tensor_tensor(out=y_sb[yi][:], in0=py[:], in1=x[:, cc, :], op=ALU.add),
                      reads=[py_b, x_b], writes=[y_b[yi]])
                kb.op("sp", lambda e, cc=cc, yi=yi, tt=tt: e.dma_start(out=outv[:, cc, tt * 512:(tt + 1) * 512], in_=y_sb[yi][:]),
                      reads=[y_b[yi]], dma=True)
        kb.finish("sp")
        kb.emit()
    return nc


def merge_host_inputs(inp, l, xs, oa, ob, oc):
    Wg = inp["w_in"][l][:, GATE0:GATE0 + 3072]
    wbr = np.stack([inp[n][l] for n in ("w_br_a", "w_br_b", "w_br_c")])
    return {
        "xT": np.ascontiguousarray(xs.T),
        "lnw": np.ascontiguousarray(inp["ln1_w"][l].reshape(8, 128).T),
        "wg": np.ascontiguousarray(Wg.reshape(8, 128, 3072).transpose(1, 0, 2)),
        "wbr": np.ascontiguousarray(wbr.reshape(3, 4, 128, 1024).transpose(2, 0, 1, 3)),
        "wo": np.ascontiguousarray(inp["w_out"][l].reshape(8, 128, 1024).transpose(1, 0, 2)),
        "oT": np.ascontiguousarray(np.stack([oa.T, ob.T, oc.T])),
    }


def build_final(T):
    nc = bass.Bass("TRN2", target_bir_lowering=False)
    xT = nc.dram_tensor("xT", [D, T], F32, kind="ExternalInput").ap()
    lnw = nc.dram_tensor("lnw", [128, 8], F32, kind="ExternalInput").ap()
    out = nc.dram_tensor("out", [D, T], F32, kind="ExternalOutput").ap()
    with ExitStack() as stack:
        kb = KB(nc, stack)
        npj = NormProj(kb, xT, lnw, T)
        y_sb = [kb.sb(f"y{i}", [128, 512], F32) for i in range(2)]; y_b = [Buf() for _ in range(2)]
        outv = out.rearrange("(c p) t -> p c t", p=128)
        npj.load(0)
        for tt in range(npj.nt):
            if tt + 1 < npj.nt:
                npj.load(tt + 1)
            npj.stats(tt)
            x = npj.x_sb[tt % 2]; x_b = npj.x_b[tt % 2]
            for cc in range(8):
                yi = cc % 2
                kb.op("dve", lambda e, cc=cc, yi=yi, x=x: e.scalar_tensor_tensor(out=y_sb[yi][:], in0=x[:, cc, :], scalar=npj.lnw32[:, cc:cc + 1],
                                                                              in1=npj.rstd_sb[:], op0=ALU.mult, op1=ALU.mult),
                      reads=[x_b, npj.rstd_b, npj.lnw32_b], writes=[y_b[yi]])
                kb.op("sp", lambda e, cc=cc, yi=yi, tt=tt: e.dma_start(out=outv[:, cc, tt * 512:(tt + 1) * 512], in_=y_sb[yi][:]),
                      reads=[y_b[yi]], dma=True)
        kb.finish("sp")
        kb.emit()
    return nc


C = 64
GN_EPS = 64e-5
LAMC = -0.6065306597126334


def build_rwkv(S):
    nc = bass.Bass("TRN2", target_bir_lowering=False)
    xT = nc.dram_tensor("xT", [D, S], F32, kind="ExternalInput").ap()
    lnw = nc.dram_tensor("lnw", [128, 8], F32, kind="ExternalInput").ap()
    w = nc.dram_tensor("w", [128, 8, 640], F32, kind="ExternalInput").ap()
    cv = nc.dram_tensor("cv", [64, 2, 9], F32, kind="ExternalInput").ap()
    cv2 = nc.dram_tensor("cv2", [128, 3], F32, kind="ExternalInput").ap()
    lr = nc.dram_tensor("lr", [128, 2, 3, 64], F32, kind="ExternalInput").ap()
    lnb = nc.dram_tensor("lnb", [64, 2, 2, 64], F32, kind="ExternalInput").ap()
    cst = nc.dram_tensor("cst", [128, 576], F32, kind="ExternalInput").ap()
    out = nc.dram_tensor("out", [S, 128], F32, kind="ExternalOutput").ap()
    with ExitStack() as stack:
        kb = KB(nc, stack)
        npj = NormProj(kb, xT, lnw, S)
        w_f = kb.sb("w_f", [128, 8, 640], F32); w_fb = Buf()
        w_b = kb.sb("w_b", [128, 8, 640], BF16); w_bb = Buf()
        cv_s = kb.sb("cv_s", [64, 2, 9], F32); cv_b = Buf()
        cv2_s = kb.sb("cv2_s", [128, 3], F32); cv2_b = Buf()
        lr_f = kb.sb("lr_f", [128, 2, 3, 64], F32); lr_fb = Buf()
        lr_b = kb.sb("lr_b", [128, 2, 3, 64], BF16); lr_bb = Buf()
        lnb_s = kb.sb("lnb_s", [64, 2, 2, 64], F32); lnb_b = Buf()
        cst_s = kb.sb("cst_s", [128, 576], F32); cst_b = Buf()
        ones_b16 = kb.sb("ones_b16", [64, 64], BF16); ones_bb = Buf()
        for dst, src, bb in ((w_f, w, w_fb), (cv_s, cv, cv_b), (cv2_s, cv2, cv2_b), (lr_f, lr, lr_fb), (lnb_s, lnb, lnb_b), (cst_s, cst, cst_b)):
            kb.op("sp", lambda e, dst=dst, src=src: e.dma_start(out=dst[:], in_=src), writes=[bb], dma=True)
        kb.op("dve", lambda e: e.tensor_copy(out=w_b[:], in_=w_f[:]), reads=[w_fb], writes=[w_bb])
        kb.op("dve", lambda e: e.tensor_copy(out=lr_b[:], in_=lr_f[:]), reads=[lr_fb], writes=[lr_bb])
        kb.op("dve", lambda e: e.memset(ones_b16[:], 1.0), writes=[ones_bb])
        ident = cst_s[:, 0:128]
        masks = cst_s[0:64, 128:448]
        ones_c = cst_s[0:64, 448:449]
        eye64 = cst_s[0:64, 512:576]
        gneps = kb.sb("gneps", [64, 1], F32); gneps_b = Buf()
        kb.op("dve", lambda e: e.memset(gneps[:], GN_EPS), writes=[gneps_b])

        NPJ = 8
        pb = [kb.sb(f"pb{i}", [128, 513], F32) for i in range(NPJ)]; pb_b = [Buf() for _ in range(NPJ)]
        for i in range(NPJ):
            kb.op("dve", lambda e, i=i: e.memset(pb[i][:, 0:1], 0.0), writes=[pb_b[i]])
        pp = [kb.ps(f"pp{i}", [128, 512]) for i in range(2)]; pp_b = [Buf() for _ in range(2)]
        pm = kb.ps("pm", [128, 512]); pm_b = Buf()
        pt = kb.ps("pt", [128, 512]); pt_b = Buf()
        pq = kb.ps("pq", [128, 512]); pq_b = Buf()
        py = kb.ps("py", [128, 512]); py_b = Buf()
        dd = kb.sb("dd", [128, 512], F32); dd_b = Buf()

        def T(name, p=64, n=512, dt=F32):
            return kb.sb(name, [p, n], dt), Buf()
        H = []
        for h in range(2):
            d = {}
            for nm in ("zr", "zk", "zv", "kk", "a", "lam", "cw", "E1", "E0", "En", "At", "Rt", "Bh", "Kh", "kf", "prod", "tmp"):
                d[nm], d[nm + "_b"] = T(f"{nm}{h}")
            d["kk2"], d["kk2_b"] = T(f"kk2{h}", dt=BF16)
            d["S"] = [kb.sb(f"S{h}_{i}", [64, 64], F32) for i in range(2)]
            d["S_b"] = [Buf() for _ in range(2)]
            d["Sw"], d["Sw_b"] = T(f"Sw{h}", n=64)
            kb.op("dve", lambda e, d=d: e.memset(d["S"][0][:], 0.0), writes=[d["S_b"][0]])
            d["si"] = 0
            H.append(d)
        zwa, zwa_b = T("zwa", p=128)
        zwa16, zwa16_b = T("zwa16", p=128, dt=BF16)
        zg, zg_b = T("zg", p=128)
        sg16, sg16_b = T("sg16", p=128, dt=BF16)
        tok, tok_b = T("tok", n=192)
        MM, MM_b = T("MM", n=320)
        PQ = [T(f"PQ{i}", n=128) for i in range(2)]
        XT = [T(f"XT{i}", n=64) for i in range(2)]
        rhs_s, rhs_b = T("rhs_s", n=64)
        u_s, u_b2 = T("u_s", n=64)
        yst, yst_b = T("yst", n=8)
        yc, yc_b = T("yc", n=64)
        ysq, ysq_b = T("ysq", n=64)
        rk_s, rk_b = T("rk_s", n=2)
        o_sb = [kb.sb(f"o{i}", [64, 128], F32) for i in range(2)]; o_b = [Buf() for _ in range(2)]

        npj.load(0)
        for tt in range(npj.nt):
            if tt + 1 < npj.nt:
                npj.load(tt + 1)
            u, u_b = npj.norm(tt)
            groups = [(h * 192 + j * 64, 64) for h in range(2) for j in range(3)] + [(384, 128), (512, 128)]
            for gi, (c0, n) in enumerate(groups):
                p = pp[gi % 2]
                kb.op("pe", [lambda e, c=c, c0=c0, n=n, p=p, u=u: e.matmul(p[0:n, :], lhsT=w_b[:, c, c0:c0 + n], rhs=u[:, c, :],
                                                                         start=(c == 0), stop=(c == 7)) for c in range(8)],
                      reads=[u_b, w_bb], writes=[pp_b[gi % 2]])
                kb.op("act", lambda e, gi=gi, n=n, p=p: e.activation(out=pb[gi][0:n, 1:513], in_=p[0:n, :], func=AF.Copy),
                      reads=[pp_b[gi % 2]], writes=[pb_b[gi]])
            def shift(gi, n, mu_ap, mu_b, dst, dst_b):
                kb.op("dve", lambda e: e.tensor_tensor(out=dd[0:n, :], in0=pb[gi][0:n, 0:512], in1=pb[gi][0:n, 1:513], op=ALU.subtract),
                      reads=[pb_b[gi]], writes=[dd_b])
                kb.op("dve", lambda e: e.scalar_tensor_tensor(out=dst[0:n, :], in0=dd[0:n, :], scalar=mu_ap, in1=pb[gi][0:n, 1:513],
                                                              op0=ALU.mult, op1=ALU.add),
                      reads=[dd_b, pb_b[gi], mu_b], writes=[dst_b])
                kb.op("dve", lambda e: e.tensor_copy(out=pb[gi][0:n, 0:1], in_=pb[gi][0:n, 512:513]), reads=[pb_b[gi]], writes=[pb_b[gi]])
            for h in range(2):
                d = H[h]
                for j, nm in enumerate(("zr", "zk", "zv")):
                    shift(h * 3 + j, 64, cv_s[:, h, j:j + 1], cv_b, d[nm], d[nm + "_b"])
            shift(6, 128, cv2_s[:, 0:1], cv2_b, zwa, zwa_b)
            shift(7, 128, cv2_s[:, 2:3], cv2_b, zg, zg_b)
            kb.op("act", lambda e: e.activation(out=zwa16[0:64, :], in_=zwa[0:64, :], func=AF.Tanh), reads=[zwa_b], writes=[zwa16_b])
            kb.op("dve", lambda e: e.tensor_copy(out=zwa16[64:128, :], in_=zwa[64:128, :]), reads=[zwa_b], writes=[zwa16_b])
            kb.op("act", lambda e: e.activation(out=sg16[:], in_=zg[:], func=AF.Sigmoid), reads=[zg_b], writes=[sg16_b])
            for h in range(2):
                d = H[h]
                kb.op("pe", lambda e, h=h: e.matmul(pp[0][0:64, :], lhsT=lr_b[0:64, h, 0, :], rhs=zwa16[0:64, :], start=True, stop=True),
                      reads=[zwa16_b, lr_bb], writes=[pp_b[0]])
                kb.op("act", lambda e, h=h, d=d: e.activation(out=d["lam"][:], in_=pp[0][0:64, :], func=AF.Sigmoid, bias=cv_s[:, h, 7:8], scale=1.0),
                      reads=[pp_b[0], cv_b], writes=[d["lam_b"]])
                kb.op("dve", lambda e, d=d: e.tensor_scalar(out=d["lam"][:], in0=d["lam"][:], scalar1=LAMC, scalar2=None, op0=ALU.mult),
                      reads=[d["lam_b"]], writes=[d["lam_b"]])
                kb.op("pe", lambda e, h=h: e.matmul(pp[1][0:64, :], lhsT=lr_b[64:128, h, 1, :], rhs=zwa16[64:128, :], start=True, stop=True),
                      reads=[zwa16_b, lr_bb], writes=[pp_b[1]])
                kb.op("act", lambda e, h=h, d=d: e.activation(out=d["a"][:], in_=pp[1][0:64, :], func=AF.Sigmoid, bias=cv_s[:, h, 8:9], scale=1.0),
                      reads=[pp_b[1], cv_b], writes=[d["a_b"]])
                kb.op("dve", lambda e, h=h, d=d: e.tensor_scalar(out=d["kk"][:], in0=d["zk"][:], scalar1=cv_s[:, h, 3:4], scalar2=None, op0=ALU.mult),
                      reads=[d["zk_b"], cv_b], writes=[d["kk_b"]])
                kb.op("act", lambda e, d=d: e.activation(out=d["kk2"][:], in_=d["kk"][:], func=AF.Square), reads=[d["kk_b"]], writes=[d["kk2_b"]])
                kb.op("pe", lambda e, d=d: e.matmul(pp[0][0:64, :], lhsT=ones_b16[:], rhs=d["kk2"][:], start=True, stop=True),
                      reads=[d["kk2_b"], ones_bb], writes=[pp_b[0]])
                kb.op("dve", lambda e, d=d: e.tensor_scalar(out=d["tmp"][:], in0=pp[0][0:64, :], scalar1=1e-12, scalar2=None, op0=ALU.max),
                      reads=[pp_b[0]], writes=[d["tmp_b"]])
                kb.op("act", lambda e, d=d: e.activation(out=d["tmp"][:], in_=d["tmp"][:], func=AF.Sqrt), reads=[d["tmp_b"]], writes=[d["tmp_b"]])
                kb.op("dve", lambda e, d=d: e.reciprocal(out=d["tmp"][:], in_=d["tmp"][:]), reads=[d["tmp_b"]], writes=[d["tmp_b"]])
                kb.op("dve", lambda e, d=d: e.tensor_tensor(out=d["kk"][:], in0=d["kk"][:], in1=d["tmp"][:], op=ALU.mult),
                      reads=[d["kk_b"], d["tmp_b"]], writes=[d["kk_b"]])
                kb.op("dve", lambda e, h=h, d=d: e.tensor_scalar(out=d["tmp"][:], in0=d["a"][:], scalar1=-1.0, scalar2=cv_s[:, h, 4:5],
                                                                 op0=ALU.add, op1=ALU.mult),
                      reads=[d["a_b"], cv_b], writes=[d["tmp_b"]])
                kb.op("dve", lambda e, d=d: e.scalar_tensor_tensor(out=d["kf"][:], in0=d["tmp"][:], scalar=1.0, in1=d["zk"][:], op0=ALU.add, op1=ALU.mult),
                      reads=[d["zk_b"], d["tmp_b"]], writes=[d["kf_b"]])
                kb.op("dve", lambda e, h=h, d=d: e.scalar_tensor_tensor(out=d["prod"][:], in0=d["zr"][:], scalar=cv_s[:, h, 6:7], in1=d["kf"][:],
                                                                        op0=ALU.mult, op1=ALU.mult),
                      reads=[d["zr_b"], d["kf_b"], cv_b], writes=[d["prod_b"]])
                for c in range(8):
                    cs = slice(c * C, (c + 1) * C)
                    kb.op("dve", lambda e, d=d, cs=cs: e.tensor_tensor_scan(out=d["cw"][:, cs], data0=cst_s[0:64, 448:512], data1=d["lam"][:, cs],
                                                                             initial=0.0, op0=ALU.mult, op1=ALU.add),
                          reads=[d["lam_b"], cst_b], writes=[d["cw_b"]] if c == 7 else [])
                kb.op("act", lambda e, d=d: e.activation(out=d["E1"][:], in_=d["cw"][:], func=AF.Exp), reads=[d["cw_b"]], writes=[d["E1_b"]])
                kb.op("act", lambda e, d=d: e.activation(out=d["En"][:], in_=d["cw"][:], func=AF.Exp, scale=-1.0), reads=[d["cw_b"]], writes=[d["En_b"]])
                kb.op("dve", lambda e, d=d: e.tensor_tensor(out=d["tmp"][:], in0=d["cw"][:], in1=d["lam"][:], op=ALU.subtract),
                      reads=[d["cw_b"], d["lam_b"]], writes=[d["tmp_b"]])
                kb.op("act", lambda e, d=d: e.activation(out=d["E0"][:], in_=d["tmp"][:], func=AF.Exp), reads=[d["tmp_b"]], writes=[d["E0_b"]])
                kb.op("dve", lambda e, d=d: e.scalar_tensor_tensor(out=d["At"][:], in0=d["kk"][:], scalar=-1.0, in1=d["E0"][:], op0=ALU.mult, op1=ALU.mult),
                      reads=[d["kk_b"], d["E0_b"]], writes=[d["At_b"]])
                kb.op("dve", lambda e, d=d: e.tensor_tensor(out=d["Rt"][:], in0=d["zr"][:], in1=d["E1"][:], op=ALU.mult),
                      reads=[d["zr_b"], d["E1_b"]], writes=[d["Rt_b"]])
                kb.op("dve", lambda e, d=d: e.tensor_tensor(out=d["tmp"][:], in0=d["kk"][:], in1=d["a"][:], op=ALU.mult),
                      reads=[d["kk_b"], d["a_b"]], writes=[d["tmp_b"]])
                kb.op("dve", lambda e, d=d: e.tensor_tensor(out=d["Bh"][:], in0=d["tmp"][:], in1=d["En"][:], op=ALU.mult),
                      reads=[d["tmp_b"], d["En_b"]], writes=[d["Bh_b"]])
                kb.op("dve", lambda e, d=d: e.tensor_tensor(out=d["Kh"][:], in0=d["kf"][:], in1=d["En"][:], op=ALU.mult),
                      reads=[d["kf_b"], d["En_b"]], writes=[d["Kh_b"]])
            for c in range(8):
                cs = slice(c * C, (c + 1) * C)
                t0 = tt * 512 + c * C
                ob = (tt * 8 + c) % 2
                for h in range(2):
                    d = H[h]
                    kb.op("pe", [lambda e, d=d, cs=cs, j=j, nm=nm: e.transpose(out=pt[0:64, j * 64:(j + 1) * 64], in_=d[nm][:, cs], identity=eye64)
                                 for j, nm in enumerate(("zv", "Bh", "Kh"))],
                          reads=[d["zv_b"], d["Bh_b"], d["Kh_b"], cst_b], writes=[pt_b])
                    kb.op("act", lambda e: e.activation(out=tok[:, 0:192], in_=pt[0:64, 0:192], func=AF.Copy), reads=[pt_b], writes=[tok_b])
                    pairs = [("Bh", "At"), ("At", "Bh"), ("Kh", "At"), ("Bh", "Rt"), ("Kh", "Rt")]
                    kb.op("pe", [lambda e, d=d, cs=cs, j=j, a=a, b=b: e.matmul(pm[0:64, j * 64:(j + 1) * 64], lhsT=d[a][:, cs], rhs=d[b][:, cs],
                                                                             start=True, stop=True) for j, (a, b) in enumerate(pairs)],
                          reads=[d["Bh_b"], d["At_b"], d["Kh_b"], d["Rt_b"]], writes=[pm_b])
                    kb.op("dve", lambda e: e.tensor_tensor(out=MM[:, 0:320], in0=pm[0:64, 0:320], in1=masks, op=ALU.mult),
                          reads=[pm_b, cst_b], writes=[MM_b])
                    kb.op("dve", lambda e: e.tensor_tensor(out=XT[0][0][:], in0=MM[:, 0:64], in1=eye64, op=ALU.add),
                          reads=[MM_b, cst_b], writes=[XT[0][1]])
                    Qc, Pc, QP_b = MM[:, 0:64], MM[:, 64:128], MM_b
                    xi = 0
                    for k in range(1, 6):
                        pq_t, pq_tb = PQ[k % 2]
                        kb.op("pe", [lambda e, Qc=Qc, Pc=Pc: e.matmul(pm[0:64, 320:384], lhsT=Pc, rhs=Qc, start=True, stop=True),
                                     lambda e, Qc=Qc, Pc=Pc: e.matmul(pm[0:64, 384:448], lhsT=Qc, rhs=Pc, start=True, stop=True)],
                              reads=[QP_b], writes=[pm_b])
                        kb.op("act", lambda e, pq_t=pq_t: e.activation(out=pq_t[:, 0:128], in_=pm[0:64, 320:448], func=AF.Copy),
                              reads=[pm_b], writes=[pq_tb])
                        Qc, Pc, QP_b = pq_t[:, 0:64], pq_t[:, 64:128], pq_tb
                        kb.op("pe", lambda e, Pc=Pc, xi=xi: e.matmul(pq[0:64, 0:64], lhsT=Pc, rhs=XT[xi][0][:], start=True, stop=True),
                              reads=[QP_b, XT[xi][1]], writes=[pq_b])
                        kb.op("dve", lambda e, xi=xi: e.tensor_tensor(out=XT[1 - xi][0][:], in0=pq[0:64, 0:64], in1=XT[xi][0][:], op=ALU.add),
                              reads=[pq_b, XT[xi][1]], writes=[XT[1 - xi][1]])
                        xi = 1 - xi
                    XTf, XTf_b = XT[xi]
                    si = d["si"]
                    S0, S0_b = d["S"][si], d["S_b"][si]
                    S1, S1_b = d["S"][1 - si], d["S_b"][1 - si]
                    d["si"] = 1 - si
                    wc = d["E1"][:, c * C + C - 1:c * C + C]
                    kb.op("dve", lambda e, d=d, S0=S0, wc=wc: e.tensor_scalar(out=d["Sw"][:], in0=S0[:], scalar1=wc, scalar2=None, op0=ALU.mult),
                          reads=[S0_b, d["E1_b"]], writes=[d["Sw_b"]])
                    kb.op("pe", [lambda e, d=d, cs=cs, S0=S0: e.matmul(pq[0:64, 64:128], lhsT=d["At"][:, cs], rhs=S0[:], start=True, stop=False),
                                 lambda e: e.matmul(pq[0:64, 64:128], lhsT=MM[:, 128:192], rhs=tok[:, 0:64], start=False, stop=True)],
                          reads=[d["At_b"], S0_b, MM_b, tok_b], writes=[pq_b])
                    kb.op("act", lambda e: e.activation(out=rhs_s[:], in_=pq[0:64, 64:128], func=AF.Copy), reads=[pq_b], writes=[rhs_b])
                    kb.op("pe", lambda e, XTf=XTf: e.matmul(pq[0:64, 128:192], lhsT=XTf[:], rhs=rhs_s[:], start=True, stop=True),
                          reads=[XTf_b, rhs_b], writes=[pq_b])
                    kb.op("act", lambda e: e.activation(out=u_s[:], in_=pq[0:64, 128:192], func=AF.Copy), reads=[pq_b], writes=[u_b2])
                    kb.op("pe", [lambda e, d=d, cs=cs, S0=S0: e.matmul(py[0:64, 0:64], lhsT=d["Rt"][:, cs], rhs=S0[:], start=True, stop=False),
                                 lambda e: e.matmul(py[0:64, 0:64], lhsT=MM[:, 192:256], rhs=u_s[:], start=False, stop=False),
                                 lambda e: e.matmul(py[0:64, 0:64], lhsT=MM[:, 256:320], rhs=tok[:, 0:64], start=False, stop=True),
                                 lambda e, d=d, cs=cs: e.matmul(py[0:64, 64:65], lhsT=d["prod"][:, cs], rhs=ones_c, start=True, stop=True),
                                 lambda e, h=h, cs=cs: e.matmul(py[0:64, 128:192], lhsT=sg16[:, cs], rhs=lr_b[:, h, 2, :], start=True, stop=True),
                                 lambda e: e.matmul(py[0:64, 192:256], lhsT=tok[:, 64:128], rhs=u_s[:], start=True, stop=False),
                                 lambda e: e.matmul(py[0:64, 192:256], lhsT=tok[:, 128:192], rhs=tok[:, 0:64], start=False, stop=True)],
                          reads=[d["Rt_b"], S0_b, MM_b, u_b2, tok_b, d["prod_b"], cst_b, sg16_b, lr_bb], writes=[py_b])
                    kb.op("dve", lambda e, S1=S1, d=d, wc=wc: e.scalar_tensor_tensor(out=S1[:], in0=py[0:64, 192:256], scalar=wc, in1=d["Sw"][:],
                                                                                    op0=ALU.mult, op1=ALU.add),
                          reads=[py_b, d["Sw_b"], d["E1_b"]], writes=[S1_b])
                    kb.op("dve", lambda e: e.tensor_reduce(out=yst[:, 0:1], in_=py[0:64, 0:64], axis=AX.X, op=ALU.add), reads=[py_b], writes=[yst_b])
                    kb.op("dve", lambda e: e.tensor_scalar(out=yst[:, 1:2], in0=yst[:, 0:1], scalar1=-1.0 / 64, scalar2=None, op0=ALU.mult),
                          reads=[yst_b], writes=[yst_b])
                    kb.op("dve", lambda e: e.tensor_scalar(out=yc[:], in0=py[0:64, 0:64], scalar1=yst[:, 1:2], scalar2=None, op0=ALU.add),
                          reads=[py_b, yst_b], writes=[yc_b])
                    kb.op("act", lambda e: e.activation(out=ysq[:], in_=yc[:], func=AF.Square, accum_out=yst[:, 2:3]), reads=[yc_b], writes=[ysq_b, yst_b])
                    kb.op("act", lambda e: e.activation(out=yst[:, 3:4], in_=yst[:, 2:3], func=AF.Sqrt, bias=gneps[:, 0:1], scale=1.0 / 64),
                          reads=[yst_b, gneps_b], writes=[yst_b])
                    kb.op("dve", lambda e: e.reciprocal(out=yst[:, 4:5], in_=yst[:, 3:4]), reads=[yst_b], writes=[yst_b])
                    kb.op("dve", lambda e, h=h: e.scalar_tensor_tensor(out=yc[:], in0=yc[:], scalar=yst[:, 4:5], in1=lnb_s[:, h, 0, :], op0=ALU.mult, op1=ALU.mult),
                          reads=[yc_b, yst_b, lnb_b], writes=[yc_b])
                    kb.op("dve", lambda e, h=h: e.tensor_tensor(out=yc[:], in0=yc[:], in1=lnb_s[:, h, 1, :], op=ALU.add), reads=[yc_b, lnb_b], writes=[yc_b])
                    kb.op("dve", lambda e: e.tensor_copy(out=rk_s[:, 0:1], in_=py[0:64, 64:65]), reads=[py_b], writes=[rk_b])
                    kb.op("dve", lambda e: e.scalar_tensor_tensor(out=yc[:], in0=tok[:, 0:64], scalar=rk_s[:, 0:1], in1=yc[:], op0=ALU.mult, op1=ALU.add),
                          reads=[tok_b, rk_b, yc_b], writes=[yc_b])
                    kb.op("dve", lambda e, h=h, ob=ob: e.tensor_tensor(out=o_sb[ob][:, h * 64:(h + 1) * 64], in0=yc[:], in1=py[0:64, 128:192], op=ALU.mult),
                          reads=[yc_b, py_b], writes=[o_b[ob]])
                kb.op("sp", lambda e, ob=ob, t0=t0: e.dma_start(out=out[t0:t0 + C, :], in_=o_sb[ob][:]), reads=[o_b[ob]], dma=True)
        kb.finish("sp")
        kb.emit()
    return nc


def rwkv_host_inputs(inp, l, b, hg, S, x_l):
    W = inp["w_in"][l]
    cols = []
    for h in range(2):
        hh = hg * 2 + h
        for j in range(3):
            cols.append(np.arange(j * 512 + hh * 64, j * 512 + (hh + 1) * 64))
    cols.append(np.arange(1536, 1792))
    cols = np.concatenate(cols)
    w = np.ascontiguousarray(W[:, cols].reshape(8, 128, 640).transpose(1, 0, 2))
    mu = inp["rwkv_mu"][l]
    cv = np.zeros((64, 2, 9), np.float32)
    for h in range(2):
        hh = hg * 2 + h
        sl = slice(hh * 64, (hh + 1) * 64)
        cv[:, h, 0] = mu[0:512][sl]; cv[:, h, 1] = mu[512:1024][sl]; cv[:, h, 2] = mu[1024:1536][sl]
        cv[:, h, 3] = inp["rwkv_k_k"][l][sl]; cv[:, h, 4] = inp["rwkv_k_a"][l][sl]
        cv[:, h, 6] = inp["rwkv_r_k"][l].reshape(-1)[sl]; cv[:, h, 7] = inp["rwkv_w0"][l][sl]; cv[:, h, 8] = inp["rwkv_a0"][l][sl]
    cv2 = np.zeros((128, 3), np.float32)
    cv2[:, 0] = mu[1536:1664]; cv2[:, 2] = mu[1664:1792]
    lr = np.zeros((128, 2, 3, 64), np.float32)
    lnb = np.zeros((64, 2, 2, 64), np.float32)
    for h in range(2):
        hh = hg * 2 + h
        sl = slice(hh * 64, (hh + 1) * 64)
        lr[0:64, h, 0] = inp["rwkv_w2"][l][:, sl]
        lr[64:128, h, 1] = inp["rwkv_a2"][l][:, sl]
        lr[:, h, 2] = inp["rwkv_g2"][l][:, sl]
        lnb[:, h, 0] = inp["rwkv_ln_w"][l][sl][None, :]
        lnb[:, h, 1] = inp["rwkv_ln_b"][l][sl][None, :]
    cst = np.zeros((128, 576), np.float32)
    cst[:, 0:128] = np.eye(128)
    i = np.arange(64)[:, None]; j = np.arange(64)[None, :]
    up_strict = (i < j).astype(np.float32)
    up_incl = (i <= j).astype(np.float32)
    lo_strict = (i > j).astype(np.float32)
    cst[0:64, 128:448] = np.concatenate([up_strict, lo_strict, up_strict, up_incl, up_incl], axis=1)
    cst[0:64, 448:512] = 1.0
    cst[0:64, 512:576] = np.eye(64)
    return {"xT": np.ascontiguousarray(x_l[b, :S].T), "lnw": np.ascontiguousarray(inp["ln1_w"][l].reshape(8, 128).T),
            "w": w, "cv": cv, "cv2": cv2, "lr": lr, "lnb": lnb, "cst": cst}


def rwkv_numpy_ref(inp, l, S):
    x = inp["x"][:, :S].astype(np.float64)
    u = x / np.sqrt((x * x).mean(-1, keepdims=True) + 1e-6) * inp["ln1_w"][l]
    proj = u @ inp["w_in"][l][:, :1792].astype(np.float64)
    prev = np.concatenate([np.zeros_like(proj[:, :1]), proj[:, :-1]], axis=1)
    rw = proj + inp["rwkv_mu"][l] * (prev - proj)
    zr, zk, zv, zw, za, zg = np.split(rw, [512, 1024, 1536, 1600, 1664], axis=-1)
    sig = lambda z: 1 / (1 + np.exp(-z))
    xw = inp["rwkv_w0"][l] + np.tanh(zw) @ inp["rwkv_w2"][l]
    decay = np.exp(LAMC * sig(xw))
    a = sig(inp["rwkv_a0"][l] + za @ inp["rwkv_a2"][l])
    g = sig(zg) @ inp["rwkv_g2"][l]
    B = 2
    kk = (zk * inp["rwkv_k_k"][l]).reshape(B, S, 8, 64)
    kk = kk / np.sqrt(np.maximum((kk * kk).sum(-1, keepdims=True), 1e-12))
    k = zk * (1 + (a - 1) * inp["rwkv_k_a"][l])
    hd = lambda z: z.reshape(B, S, 8, 64)
    r_h, k_h, v_h, a_h, w_h = hd(zr), hd(k), hd(zv), hd(a), hd(decay)
    y = np.zeros((B, S, 8, 64))
    st = np.zeros((B, 8, 64, 64))
    for t in range(S):
        sa = np.einsum('bhij,bhj->bhi', st, -kk[:, t])
        st = st * w_h[:, t][:, :, None, :] + sa[..., None] * (kk[:, t] * a_h[:, t])[:, :, None, :] + v_h[:, t][..., None] * k_h[:, t][:, :, None, :]
        y[:, t] = np.einsum('bhij,bhj->bhi', st, r_h[:, t])
    mu_ = y.mean(-1, keepdims=True); var = ((y - mu_) ** 2).mean(-1, keepdims=True)
    yn = ((y - mu_) / np.sqrt(var + GN_EPS)).reshape(B, S, 512) * inp["rwkv_ln_w"][l] + inp["rwkv_ln_b"][l]
    bonus = ((r_h * k_h * inp["rwkv_r_k"][l]).sum(-1, keepdims=True) * v_h).reshape(B, S, 512)
    return (yn + bonus) * g


NEG = -30000.0
NB0 = 1792
NCOL = 774


def kb_barrier(kb):
    for eng in ("pe", "dve", "act", "sp"):
        needs = {}
        for key, val in kb.cnt.items():
            if val > 0:
                kb._need(eng, needs, (key, val))
        kb._flush_waits(eng, needs)


O_T0, O_T1 = 0, 256
O_W = 512
O_CM = 1792
O_RV = 1800
O_FAR = 1801
O_UW = 1803
O_FW = 2059
O_ID = 2315
NCST = 2443


def build_nsa(S):
    nc = bass.Bass("TRN2", target_bir_lowering=False)
    xT = nc.dram_tensor("xT", [D, S], F32, kind="ExternalInput").ap()
    lnw = nc.dram_tensor("lnw", [128, 8], F32, kind="ExternalInput").ap()
    w = nc.dram_tensor("w", [128, 8, NCOL], F32, kind="ExternalInput").ap()
    w1 = nc.dram_tensor("w1", [128, 32, 256], F32, kind="ExternalInput").ap()
    peT = nc.dram_tensor("peT", [128, 32], F32, kind="ExternalInput").ap()
    w2 = nc.dram_tensor("w2", [128, 2, 192], F32, kind="ExternalInput").ap()
    cst = nc.dram_tensor("cst", [128, NCST], F32, kind="ExternalInput").ap()
    out = nc.dram_tensor("out", [S, 128], F32, kind="ExternalOutput").ap()
    nblk = S // 128
    with ExitStack() as stack:
        kb = KB(nc, stack)
        q4T = kb.sb("q4T", [128, 2, S], BF16); q4T_b = Buf()
        kcvcT = kb.sb("kcvcT", [128, S], BF16); kcvc_b = Buf()
        ksT = kb.sb("ksT", [128, S], BF16); ksT_b = Buf()
        kwT = kb.sb("kwT", [128, S], BF16); kwT_b = Buf()
        vs_tm = kb.sb("vs_tm", [128, nblk, 65], BF16); vs_b = Buf()
        vw_tm = kb.sb("vw_tm", [128, nblk, 65], BF16); vw_b = Buf()
        gt_tm = kb.sb("gt_tm", [128, nblk, 6], F32); gt_b = Buf()
        cst_s = kb.sb("cst_s", [128, NCST], F32); cst_b = Buf()
        id_b16 = kb.sb("id_b16", [128, 128], BF16); id_bb = Buf()
        kb.op("sp", lambda e: e.dma_start(out=cst_s[:], in_=cst), writes=[cst_b], dma=True)
        kb.op("dve", lambda e: e.tensor_copy(out=id_b16[:], in_=cst_s[:, O_ID:O_ID + 128]), reads=[cst_b], writes=[id_bb])
        kb.op("dve", lambda e: e.memset(vs_tm[:, :, 64:65], 1.0), writes=[vs_b])
        kb.op("dve", lambda e: e.memset(vw_tm[:, :, 64:65], 1.0), writes=[vw_b])
        pp = [kb.ps(f"pp{i}", [128, 512]) for i in range(2)]; pp_b = [Buf() for _ in range(2)]
        with ExitStack() as st1:
            kb.stack = st1
            npj = NormProj(kb, xT, lnw, S)
            stg = kb.sb("stg", [128, 2048], F32); stg_b = Buf()
            w_b = kb.sb("w_b", [128, 8, NCOL], BF16); w_bb = Buf()
            for c in range(8):
                kb.op("sp", lambda e, c=c: e.dma_start(out=stg[:, 0:NCOL], in_=w[:, c, :]), writes=[stg_b], dma=True)
                kb.op("dve", lambda e, c=c: e.tensor_copy(out=w_b[:, c, :], in_=stg[:, 0:NCOL]), reads=[stg_b], writes=[w_bb])
            fm = [(q4T[:, 0], q4T_b), (q4T[:, 1], q4T_b), (kcvcT, kcvc_b), (ksT, ksT_b), (kwT, kwT_b)]
            npj.load(0)
            for tt in range(npj.nt):
                if tt + 1 < npj.nt:
                    npj.load(tt + 1)
                u, u_b = npj.norm(tt)
                for gi, (dst, dst_b) in enumerate(fm):
                    p = pp[gi % 2]
                    kb.op("pe", [lambda e, c=c, gi=gi, p=p, u=u: e.matmul(p[:], lhsT=w_b[:, c, gi * 128:(gi + 1) * 128], rhs=u[:, c, :],
                                                                        start=(c == 0), stop=(c == 7)) for c in range(8)],
                          reads=[u_b, w_bb], writes=[pp_b[gi % 2]])
                    kb.op("act", lambda e, dst=dst, p=p, tt=tt: e.activation(out=dst[:, tt * 512:(tt + 1) * 512], in_=p[:], func=AF.Copy),
                          reads=[pp_b[gi % 2]], writes=[dst_b])
                for sbk in range(4):
                    blk = tt * 4 + sbk
                    p = pp[sbk % 2]
                    kb.op("pe", [lambda e, c=c, sbk=sbk, p=p, u=u: e.matmul(p[:, 0:134], lhsT=u[:, c, sbk * 128:(sbk + 1) * 128], rhs=w_b[:, c, 640:774],
                                                                          start=(c == 0), stop=(c == 7)) for c in range(8)],
                          reads=[u_b, w_bb], writes=[pp_b[sbk % 2]])
                    kb.op("dve", lambda e, p=p, blk=blk: e.tensor_copy(out=vs_tm[:, blk, 0:64], in_=p[:, 0:64]), reads=[pp_b[sbk % 2]], writes=[vs_b])
                    kb.op("dve", lambda e, p=p, blk=blk: e.tensor_copy(out=vw_tm[:, blk, 0:64], in_=p[:, 64:128]), reads=[pp_b[sbk % 2]], writes=[vw_b])
                    kb.op("act", lambda e, p=p, blk=blk: e.activation(out=gt_tm[:, blk, :], in_=p[:, 128:134], func=AF.Sigmoid),
                          reads=[pp_b[sbk % 2]], writes=[gt_b])
            kb_barrier(kb)
        kb.stack = stack
        ncmp = S // 16 - 1
        kcmpT = kb.sb("kcmpT", [128, 512], BF16); kcmp_b = Buf()
        vcmp = kb.sb("vcmp", [128, 4, 64], BF16); vcmp_b = Buf()
        kb.op("dve", lambda e: e.memset(kcmpT[:], 0.0), writes=[kcmp_b])
        kb.op("dve", lambda e: e.memset(vcmp[:], 0.0), writes=[vcmp_b])
        with ExitStack() as st2:
            kb.stack = st2
            stg2 = kb.sb("stg2", [128, 2048], F32); stg2_b = Buf()
            w1_b = kb.sb("w1_b", [128, 32, 256], BF16); w1_bb = Buf()
            for l0 in range(0, 32, 8):
                kb.op("sp", lambda e, l0=l0: e.dma_start(out=stg2[:, :].rearrange("p (a b) -> p a b", a=8), in_=w1[:, l0:l0 + 8, :]),
                      writes=[stg2_b], dma=True)
                kb.op("dve", lambda e, l0=l0: e.tensor_copy(out=w1_b[:, l0:l0 + 8, :], in_=stg2[:, :].rearrange("p (a b) -> p a b", a=8)),
                      reads=[stg2_b], writes=[w1_bb])
            pe_f = kb.sb("pe_f", [128, 32], F32); pe_fb = Buf()
            pe_b = kb.sb("pe_b", [128, 32], BF16); pe_bb = Buf()
            w2_f = kb.sb("w2_f", [128, 2, 192], F32); w2_fb = Buf()
            w2_b = kb.sb("w2_b", [128, 2, 192], BF16); w2_bb = Buf()
            kb.op("sp", lambda e: e.dma_start(out=pe_f[:], in_=peT), writes=[pe_fb], dma=True)
            kb.op("sp", lambda e: e.dma_start(out=w2_f[:], in_=w2), writes=[w2_fb], dma=True)
            kb.op("dve", lambda e: e.tensor_copy(out=pe_b[:], in_=pe_f[:]), reads=[pe_fb], writes=[pe_bb])
            kb.op("dve", lambda e: e.tensor_copy(out=w2_b[:], in_=w2_f[:]), reads=[w2_fb], writes=[w2_bb])
            hid = kb.sb("hid", [128, 2, 2, 512], BF16); hid_b = Buf()
            kb.op("dve", lambda e: e.memset(hid[:], 0.0), writes=[hid_b])
            pcn = kb.sb("pcn", [128, 4], F32); pcn_b = Buf()
            for br in range(2):
                ps_ = slice(br * 64, (br + 1) * 64)
                for mc in range(2):
                    kb.op("pe", [lambda e, l=l, mc=mc, ps_=ps_: e.matmul(pp[1][:, 0:1], lhsT=w1_b[ps_, l, mc * 128:(mc + 1) * 128], rhs=pe_b[ps_, l:l + 1],
                                                                       start=(l == 0), stop=(l == 31)) for l in range(32)],
                          reads=[w1_bb, pe_bb], writes=[pp_b[1]])
                    kb.op("dve", lambda e, br=br, mc=mc: e.tensor_copy(out=pcn[:, br * 2 + mc:br * 2 + mc + 1], in_=pp[1][:, 0:1]),
                          reads=[pp_b[1]], writes=[pcn_b])
                    kb.op("pe", [lambda e, l=l, mc=mc, ps_=ps_: e.matmul(pp[0][:, 0:ncmp], lhsT=w1_b[ps_, l, mc * 128:(mc + 1) * 128],
                                                                       rhs=kcvcT[ps_, l:l + 16 * (ncmp - 1) + 1:16], start=(l == 0), stop=(l == 31))
                                 for l in range(32)],
                          reads=[w1_bb, kcvc_b], writes=[pp_b[0]])
                    kb.op("act", lambda e, br=br, mc=mc: e.activation(out=hid[:, br, mc, 0:ncmp], in_=pp[0][:, 0:ncmp], func=AF.Gelu,
                                                                      bias=pcn[:, br * 2 + mc:br * 2 + mc + 1], scale=1.0),
                          reads=[pp_b[0], pcn_b], writes=[hid_b])
            kb.op("pe", [lambda e, mc=mc: e.matmul(pp[0][:, 0:ncmp], lhsT=w2_b[:, mc, 0:128], rhs=hid[:, 0, mc, 0:ncmp], start=(mc == 0), stop=(mc == 1))
                         for mc in range(2)], reads=[w2_bb, hid_b], writes=[pp_b[0]])
            kb.op("act", lambda e: e.activation(out=kcmpT[:, 0:ncmp], in_=pp[0][:, 0:ncmp], func=AF.Copy), reads=[pp_b[0]], writes=[kcmp_b])
            for cc in range((ncmp + 127) // 128):
                n = min(128, ncmp - cc * 128)
                kb.op("pe", [lambda e, mc=mc, cc=cc, n=n: e.matmul(pp[1][0:n, 0:64], lhsT=hid[:, 1, mc, cc * 128:cc * 128 + n], rhs=w2_b[:, mc, 128:192],
                                                                 start=(mc == 0), stop=(mc == 1)) for mc in range(2)],
                      reads=[w2_bb, hid_b], writes=[pp_b[1]])
                kb.op("dve", lambda e, cc=cc, n=n: e.tensor_copy(out=vcmp[0:n, cc, :], in_=pp[1][0:n, 0:64]), reads=[pp_b[1]], writes=[vcmp_b])
            kb_barrier(kb)
        kb.stack = stack
        psc = [kb.ps(f"psc{i}", [128, 512]) for i in range(2)]; psc_b = [Buf() for _ in range(2)]
        ptr = kb.ps("ptr", [128, 512], BF16); ptr_b = Buf()
        po = kb.ps("po", [128, 512]); po_b = Buf()
        e_all = kb.sb("e_all", [128, S], BF16); e_ab = Buf()
        eT = [kb.sb(f"eT{i}", [128, 512], BF16) for i in range(2)]; eT_b = [Buf() for _ in range(2)]
        s_c = kb.sb("s_c", [128, 512], F32); s_cb = Buf()
        pc = kb.sb("pc", [128, 512], F32); pc_b = Buf()
        pc16 = kb.sb("pc16", [128, 512], BF16); pc16_b = Buf()
        pcsum = kb.sb("pcsum", [128, 516], F32); pcsum_b = Buf()
        imp = kb.sb("imp", [128, 128], F32); imp_b = Buf()
        imp2 = kb.sb("imp2", [128, 128], F32); imp2_b = Buf()
        sel = kb.sb("sel", [128, 128], BF16); sel_b = Buf()
        m8 = kb.sb("m8", [128, 16], F32); m8_b = Buf()
        s_near = kb.sb("s_near", [128, 640], F32); sn_b = Buf()
        mx = kb.sb("mx", [128, 24], F32); mx_b = Buf()
        st = kb.sb("st", [128, 16], F32); st_b = Buf()
        ocmp = kb.sb("ocmp", [128, 128], F32); ocmp_b = Buf()
        osel = kb.sb("osel", [128, 128], F32); osel_b = Buf()
        o_sb = [kb.sb(f"o{i}", [128, 128], F32) for i in range(2)]; o_b = [Buf() for _ in range(2)]
        kb.op("dve", lambda e: e.memset(pcsum[:], 0.0), writes=[pcsum_b])
        own = None

        def transposes_pv(e_src, e_src_b, ncols, v_list, v_b, out_ps_cols, nvcol):
            nch = (ncols + 127) // 128
            done = 0
            first = True
            while done < nch:
                g = min(4, nch - done)
                ei = (done // 4) % 2
                chunks = []
                for j in range(g):
                    c0 = (done + j) * 128
                    n = min(128, ncols - c0)
                    chunks.append((j, c0, n))
                kb.op("pe", [lambda e, j=j, c0=c0, n=n: e.transpose(out=ptr[0:n, j * 128:(j + 1) * 128], in_=e_src[:, c0:c0 + n], identity=id_b16[:])
                             for (j, c0, n) in chunks], reads=[e_src_b, id_bb], writes=[ptr_b])
                kb.op("dve", lambda e, g=g, ei=ei: e.tensor_copy(out=eT[ei][:, 0:g * 128], in_=ptr[:, 0:g * 128]), reads=[ptr_b], writes=[eT_b[ei]])
                last = (done + g == nch)
                kb.op("pe", [lambda e, j=j, n=n, ei=ei, vi=done + j, st_=(first and j == 0), sp_=(last and j == g - 1): e.matmul(
                    po[:, out_ps_cols:out_ps_cols + nvcol], lhsT=eT[ei][0:n, j * 128:(j + 1) * 128], rhs=v_list[vi][0:n], start=st_, stop=sp_)
                    for (j, c0, n) in chunks], reads=[eT_b[ei], v_b], writes=[po_b])
                first = False
                done += g

        for i in range(nblk):
            qs = slice(i * 128, (i + 1) * 128)
            ob = i % 2
            ncol = min(8 * i + 7, ncmp)
            kb.op("dve", lambda e: e.memset(pcsum[:, 0:512], 0.0), writes=[pcsum_b])
            for pr in range(2):
                for hh in range(2):
                    hs = slice(hh * 64, (hh + 1) * 64)
                    p = psc[(pr * 2 + hh) % 2]; p_b = psc_b[(pr * 2 + hh) % 2]
                    kb.op("pe", lambda e, p=p, pr=pr, hs=hs, qs=qs, ncol=ncol: e.matmul(p[:, 0:ncol], lhsT=q4T[hs, pr, qs], rhs=kcmpT[hs, 0:ncol],
                                                                                      start=True, stop=True),
                          reads=[q4T_b, kcmp_b], writes=[p_b])
                    kb.op("dve", lambda e, p=p, ncol=ncol: e.tensor_scalar(out=s_c[:, 0:ncol], in0=p[:, 0:ncol], scalar1=0.125, scalar2=None, op0=ALU.mult),
                          reads=[p_b], writes=[s_cb])
                    npart = 8 if i > 0 else 7
                    kb.op("dve", lambda e, ncol=ncol, npart=npart: e.tensor_tensor(out=s_c[:, ncol - npart:ncol], in0=s_c[:, ncol - npart:ncol],
                                                                                 in1=cst_s[:, O_CM + 8 - npart:O_CM + 8], op=ALU.add),
                          reads=[s_cb, cst_b], writes=[s_cb])
                    kb.op("dve", lambda e, ncol=ncol: e.tensor_reduce(out=st[:, 0:1], in_=s_c[:, 0:ncol], axis=AX.X, op=ALU.max), reads=[s_cb], writes=[st_b])
                    kb.op("dve", lambda e: e.tensor_scalar(out=st[:, 1:2], in0=st[:, 0:1], scalar1=-1.0, scalar2=None, op0=ALU.mult), reads=[st_b], writes=[st_b])
                    kb.op("act", lambda e, ncol=ncol: e.activation(out=pc[:, 0:ncol], in_=s_c[:, 0:ncol], func=AF.Exp, bias=st[:, 1:2], scale=1.0,
                                                                  accum_out=st[:, 2:3]), reads=[s_cb, st_b], writes=[pc_b, st_b])
                    kb.op("dve", lambda e: e.reciprocal(out=st[:, 3:4], in_=st[:, 2:3]), reads=[st_b], writes=[st_b])
                    if i == 0:
                        kb.op("dve", lambda e: e.tensor_tensor(out=st[:, 3:4], in0=st[:, 3:4], in1=cst_s[:, O_RV:O_RV + 1], op=ALU.mult),
                              reads=[st_b, cst_b], writes=[st_b])
                    kb.op("dve", lambda e, ncol=ncol: e.tensor_scalar(out=pc[:, 0:ncol], in0=pc[:, 0:ncol], scalar1=st[:, 3:4], scalar2=None, op0=ALU.mult),
                          reads=[pc_b, st_b], writes=[pc_b])
                    kb.op("dve", lambda e, ncol=ncol: e.tensor_tensor(out=pcsum[:, 0:ncol], in0=pcsum[:, 0:ncol], in1=pc[:, 0:ncol], op=ALU.add),
                          reads=[pc_b, pcsum_b], writes=[pcsum_b])
                    if pr == 0:
                        kb.op("dve", lambda e, ncol=ncol: e.tensor_copy(out=pc16[:, 0:ncol], in_=pc[:, 0:ncol]), reads=[pc_b], writes=[pc16_b])
                        nch = (ncol + 127) // 128
                        transposes_pv(pc16, pc16_b, ncol, [vcmp[:, cc, :] for cc in range(nch)], vcmp_b, hh * 64, 64)
                        kb.op("act", lambda e, hh=hh: e.activation(out=ocmp[:, hh * 64:(hh + 1) * 64], in_=po[:, hh * 64:(hh + 1) * 64], func=AF.Copy),
                              reads=[po_b], writes=[ocmp_b])
            pv4 = pcsum[:, 0:512].rearrange("p (n f) -> p n f", f=4)
            kb.op("dve", lambda e, pv4=pv4: e.tensor_reduce(out=imp[:], in_=pv4, axis=AX.X, op=ALU.add), reads=[pcsum_b], writes=[imp_b])
            kb.op("dve", lambda e: e.tensor_tensor(out=imp[:, 1:128], in0=imp[:, 1:128], in1=pcsum[:, 3:3 + 4 * 126 + 1:4], op=ALU.add),
                  reads=[pcsum_b, imp_b], writes=[imp_b])
            u0 = O_UW + 126 - 2 * i
            f0 = O_FW + 126 - 2 * i
            if i <= 63:
                kb.op("dve", lambda e, u0=u0: e.tensor_tensor(out=imp2[:], in0=imp[:], in1=cst_s[:, u0:u0 + 128], op=ALU.min),
                      reads=[imp_b, cst_b], writes=[imp2_b])
                kb.op("dve", lambda e, f0=f0: e.tensor_tensor(out=imp2[:], in0=imp2[:], in1=cst_s[:, f0:f0 + 128], op=ALU.max),
                      reads=[imp2_b, cst_b], writes=[imp2_b])
            kb.op("dve", lambda e: e.memset(imp2[:, 0:1], 1e9), reads=[imp2_b], writes=[imp2_b])
            kb.op("dve", lambda e: e.max(out=m8[:, 0:8], in_=imp2[:]), reads=[imp2_b], writes=[m8_b])
            kb.op("dve", lambda e: e.match_replace(out=imp[:], in_to_replace=m8[:, 0:8], in_values=imp2[:], imm_value=-2.0),
                  reads=[imp2_b, m8_b], writes=[imp_b])
            kb.op("dve", lambda e: e.max(out=m8[:, 8:16], in_=imp[:]), reads=[imp_b], writes=[m8_b])
            kb.op("dve", lambda e: e.tensor_scalar(out=sel[:], in0=imp2[:], scalar1=m8[:, 15:16], scalar2=None, op0=ALU.is_ge),
                  reads=[imp2_b, m8_b], writes=[sel_b])
            nfar = max(0, i - 1) * 128
            nnear = 128 * min(i + 1, 2)
            ngrp = (nfar + 511) // 512
            for hh in range(2):
                hs = slice(hh * 64, (hh + 1) * 64)
                lq = q4T[hs, 0, qs]
                pn = psc[0]
                kb.op("pe", lambda e, lq=lq, hs=hs, i=i, nnear=nnear, pn=pn: e.matmul(pn[:, 0:nnear], lhsT=lq, rhs=ksT[hs, (i + 1) * 128 - nnear:(i + 1) * 128],
                                                                                    start=True, stop=True), reads=[q4T_b, ksT_b], writes=[psc_b[0]])
                if i > 0:
                    kb.op("dve", lambda e, hh=hh, pn=pn: e.scalar_tensor_tensor(out=s_near[:, 0:128], in0=pn[:, 0:128], scalar=0.125,
                                                                               in1=cst_s[:, O_T1 + hh * 128:O_T1 + (hh + 1) * 128], op0=ALU.mult, op1=ALU.add),
                          reads=[psc_b[0], cst_b], writes=[sn_b])
                kb.op("dve", lambda e, hh=hh, pn=pn, nnear=nnear: e.scalar_tensor_tensor(out=s_near[:, nnear - 128:nnear], in0=pn[:, nnear - 128:nnear], scalar=0.125,
                                                                                       in1=cst_s[:, O_T0 + hh * 128:O_T0 + (hh + 1) * 128], op0=ALU.mult, op1=ALU.add),
                      reads=[psc_b[0], cst_b], writes=[sn_b])
                kb.op("dve", lambda e, nnear=nnear: e.tensor_reduce(out=mx[:, 0:1], in_=s_near[:, 0:nnear], axis=AX.X, op=ALU.max), reads=[sn_b], writes=[mx_b])
                for G in range(ngrp):
                    n = min(512, nfar - G * 512)
                    p = psc[1]
                    kb.op("pe", lambda e, lq=lq, hs=hs, G=G, n=n, p=p: e.matmul(p[:, 0:n], lhsT=lq, rhs=ksT[hs, G * 512:G * 512 + n], start=True, stop=True),
                          reads=[q4T_b, ksT_b], writes=[psc_b[1]])
                    kb.op("dve", lambda e, G=G, n=n, p=p: e.tensor_reduce(out=mx[:, 1 + G:2 + G], in_=p[:, 0:n], axis=AX.X, op=ALU.max),
                          reads=[psc_b[1]], writes=[mx_b])
                if ngrp > 0:
                    kb.op("dve", lambda e, ngrp=ngrp: e.tensor_reduce(out=mx[:, 20:21], in_=mx[:, 1:1 + ngrp], axis=AX.X, op=ALU.max), reads=[mx_b], writes=[mx_b])
                    kb.op("dve", lambda e, hh=hh: e.tensor_scalar(out=mx[:, 20:21], in0=mx[:, 20:21], scalar1=0.125, scalar2=cst_s[:, O_FAR + hh:O_FAR + hh + 1],
                                                                  op0=ALU.mult, op1=ALU.add), reads=[mx_b, cst_b], writes=[mx_b])
                    kb.op("dve", lambda e: e.tensor_tensor(out=mx[:, 0:1], in0=mx[:, 0:1], in1=mx[:, 20:21], op=ALU.max), reads=[mx_b], writes=[mx_b])
                kb.op("dve", lambda e: e.tensor_scalar(out=mx[:, 21:22], in0=mx[:, 0:1], scalar1=-1.0, scalar2=None, op0=ALU.mult), reads=[mx_b], writes=[mx_b])
                kb.op("dve", lambda e, hh=hh: e.tensor_tensor(out=mx[:, 22:23], in0=mx[:, 21:22], in1=cst_s[:, O_FAR + hh:O_FAR + hh + 1], op=ALU.add),
                      reads=[mx_b, cst_b], writes=[mx_b])
                for G in range(ngrp):
                    n = min(512, nfar - G * 512)
                    p = psc[G % 2]
                    kb.op("pe", lambda e, lq=lq, hs=hs, G=G, n=n, p=p: e.matmul(p[:, 0:n], lhsT=lq, rhs=ksT[hs, G * 512:G * 512 + n], start=True, stop=True),
                          reads=[q4T_b, ksT_b], writes=[psc_b[G % 2]])
                    kb.op("act", lambda e, G=G, n=n, p=p: e.activation(out=e_all[:, G * 512:G * 512 + n], in_=p[:, 0:n], func=AF.Exp, bias=mx[:, 22:23], scale=0.125),
                          reads=[psc_b[G % 2], mx_b], writes=[e_ab])
                kb.op("act", lambda e, nfar=nfar, nnear=nnear: e.activation(out=e_all[:, nfar:nfar + nnear], in_=s_near[:, 0:nnear], func=AF.Exp, bias=mx[:, 21:22], scale=1.0),
                      reads=[sn_b, mx_b], writes=[e_ab])
                nk = nfar + nnear
                nb64 = nk // 64
                ev = e_all[:, 0:nk].rearrange("p (n k) -> p n k", k=64)
                kb.op("dve", lambda e, ev=ev, nb64=nb64: e.tensor_tensor(out=ev, in0=ev, in1=sel[:, 0:nb64].unsqueeze(2).broadcast_to([128, nb64, 64]), op=ALU.mult),
                      reads=[e_ab, sel_b], writes=[e_ab])
                transposes_pv(e_all, e_ab, nk, [vs_tm[:, kbk, :] for kbk in range(nk // 128)], vs_b, 128, 65)
                kb.op("dve", lambda e: e.reciprocal(out=st[:, 8:9], in_=po[:, 128 + 64:128 + 65]), reads=[po_b], writes=[st_b])
                kb.op("dve", lambda e, hh=hh: e.tensor_scalar(out=osel[:, hh * 64:(hh + 1) * 64], in0=po[:, 128:192], scalar1=st[:, 8:9], scalar2=None, op0=ALU.mult),
                      reads=[po_b, st_b], writes=[osel_b])
                nwb = min(i + 1, 5)
                k0 = (i + 1 - nwb) * 128
                pw = [psc[0], psc[1]]
                if nwb > 1:
                    kb.op("pe", lambda e, lq=lq, hs=hs, k0=k0, nwb=nwb: e.matmul(pw[0][:, 0:(nwb - 1) * 128], lhsT=lq, rhs=kwT[hs, k0:k0 + (nwb - 1) * 128], start=True, stop=True),
                          reads=[q4T_b, kwT_b], writes=[psc_b[0]])
                kb.op("pe", lambda e, lq=lq, hs=hs, i=i: e.matmul(pw[1][:, 0:128], lhsT=lq, rhs=kwT[hs, i * 128:(i + 1) * 128], start=True, stop=True),
                      reads=[q4T_b, kwT_b], writes=[psc_b[1]])
                for j in range(nwb):
                    dd_ = nwb - 1 - j
                    src = pw[0][:, j * 128:(j + 1) * 128] if j < nwb - 1 else pw[1][:, 0:128]
                    src_b = psc_b[0] if j < nwb - 1 else psc_b[1]
                    wo_ = O_W + (hh * 5 + dd_) * 128
                    kb.op("dve", lambda e, j=j, src=src, wo_=wo_: e.scalar_tensor_tensor(out=s_near[:, j * 128:(j + 1) * 128], in0=src, scalar=0.125,
                                                                                      in1=cst_s[:, wo_:wo_ + 128], op0=ALU.mult, op1=ALU.add),
                          reads=[src_b, cst_b], writes=[sn_b])
                nw = nwb * 128
                kb.op("dve", lambda e, nw=nw: e.tensor_reduce(out=st[:, 4:5], in_=s_near[:, 0:nw], axis=AX.X, op=ALU.max), reads=[sn_b], writes=[st_b])
                kb.op("dve", lambda e: e.tensor_scalar(out=st[:, 5:6], in0=st[:, 4:5], scalar1=-1.0, scalar2=None, op0=ALU.mult), reads=[st_b], writes=[st_b])
                kb.op("act", lambda e, nw=nw: e.activation(out=e_all[:, 0:nw], in_=s_near[:, 0:nw], func=AF.Exp, bias=st[:, 5:6], scale=1.0),
                      reads=[sn_b, st_b], writes=[e_ab])
                transposes_pv(e_all, e_ab, nw, [vw_tm[:, i + 1 - nwb + j, :] for j in range(nwb)], vw_b, 256, 65)
                kb.op("dve", lambda e: e.reciprocal(out=st[:, 9:10], in_=po[:, 256 + 64:256 + 65]), reads=[po_b], writes=[st_b])
                oc = o_sb[ob][:, hh * 64:(hh + 1) * 64]
                kb.op("dve", lambda e, hh=hh, i=i, oc=oc: e.tensor_scalar(out=oc, in0=po[:, 256:320], scalar1=st[:, 9:10], scalar2=gt_tm[:, i, hh * 3 + 2:hh * 3 + 3],
                                                                        op0=ALU.mult, op1=ALU.mult), reads=[po_b, st_b, gt_b], writes=[o_b[ob]])
                kb.op("dve", lambda e, hh=hh, i=i, oc=oc: e.scalar_tensor_tensor(out=oc, in0=osel[:, hh * 64:(hh + 1) * 64], scalar=gt_tm[:, i, hh * 3 + 1:hh * 3 + 2],
                                                                               in1=oc, op0=ALU.mult, op1=ALU.add), reads=[osel_b, gt_b, o_b[ob]], writes=[o_b[ob]])
                kb.op("dve", lambda e, hh=hh, i=i, oc=oc: e.scalar_tensor_tensor(out=oc, in0=ocmp[:, hh * 64:(hh + 1) * 64], scalar=gt_tm[:, i, hh * 3:hh * 3 + 1],
                                                                               in1=oc, op0=ALU.mult, op1=ALU.add), reads=[ocmp_b, gt_b, o_b[ob]], writes=[o_b[ob]])
            kb.op("sp", lambda e, ob=ob, i=i: e.dma_start(out=out[i * 128:(i + 1) * 128, :], in_=o_sb[ob][:]), reads=[o_b[ob]], dma=True)
        kb.finish("sp")
        kb.emit()
    return nc


def nsa_host_inputs(inp, l, b, hg, S, x_l):
    g = hg // 2
    W = inp["w_in"][l]
    own_pair = hg % 2
    qcols = lambda pair: np.arange(NB0 + (g * 4 + pair * 2) * 64, NB0 + (g * 4 + pair * 2 + 2) * 64)
    gsl = lambda off: np.arange(NB0 + off + g * 64, NB0 + off + (g + 1) * 64)
    cols = np.concatenate([qcols(own_pair), qcols(1 - own_pair), gsl(512), gsl(640), gsl(768), gsl(768), gsl(1024), gsl(1024),
                           gsl(896), gsl(1152), np.arange(NB0 + 1280 + hg * 6, NB0 + 1280 + hg * 6 + 6)])
    assert len(cols) == NCOL
    w = np.ascontiguousarray(W[:, cols].reshape(8, 128, NCOL).transpose(1, 0, 2))
    w1 = np.ascontiguousarray(np.concatenate([inp["nsa_ck_w1"][l].reshape(32, 64, 256).transpose(1, 0, 2),
                                              inp["nsa_cv_w1"][l].reshape(32, 64, 256).transpose(1, 0, 2)], axis=0))
    peT = np.ascontiguousarray(np.concatenate([inp["nsa_pe_k"][l].T, inp["nsa_pe_v"][l].T], axis=0))
    w2k = inp["nsa_ck_w2"][l].reshape(2, 128, 64).transpose(1, 0, 2)
    w2v = inp["nsa_cv_w2"][l].reshape(2, 128, 64).transpose(1, 0, 2)
    w2 = np.ascontiguousarray(np.concatenate([w2k, w2k, w2v], axis=2))
    tbl = inp["rel_bias"][:, :8]
    cst = np.zeros((128, NCST), np.float32)
    q = np.arange(128)[:, None]; k = np.arange(128)[None, :]
    for h in range(2):
        head = hg * 2 + h
        d0 = q - k
        cst[:, O_T0 + h * 128:O_T0 + (h + 1) * 128] = np.where(d0 >= 0, tbl[t5_bucket_np(d0), head], NEG)
        cst[:, O_T1 + h * 128:O_T1 + (h + 1) * 128] = tbl[t5_bucket_np(d0 + 128), head]
        for dd in range(5):
            dist = d0 + 128 * dd
            cst[:, O_W + (h * 5 + dd) * 128:O_W + (h * 5 + dd + 1) * 128] = np.where((dist >= 0) & (dist < 512), tbl[t5_bucket_np(dist), head], NEG)
        cst[:, O_FAR + h] = tbl[31, head]
    j = np.arange(8)[None, :]
    cst[:, O_CM:O_CM + 8] = np.where(16 * j + 15 <= q, 0.0, NEG)
    cst[:, O_RV] = (np.arange(128) >= 31).astype(np.float32)
    m = np.arange(256)[None, :]
    hi = (np.arange(128)[:, None] >= 64).astype(np.int64)
    cst[:, O_UW:O_UW + 256] = np.where(m <= 126 + hi, 1e30, -1.0)
    cst[:, O_FW:O_FW + 256] = np.where((m == 126 + hi) | (m == 125 + hi), 1e9, -1e30)
    cst[:, O_ID:O_ID + 128] = np.eye(128)
    return {"xT": np.ascontiguousarray(x_l[b, :S].T), "lnw": np.ascontiguousarray(inp["ln1_w"][l].reshape(8, 128).T),
            "w": w, "w1": w1, "peT": peT, "w2": w2, "cst": cst}


def nsa_numpy_ref(inp, l, S, b=0):
    from scipy.special import erf
    x = inp["x"][b, :S].astype(np.float64)
    u = x / np.sqrt((x * x).mean(-1, keepdims=True) + 1e-6) * inp["ln1_w"][l]
    proj = u @ inp["w_in"][l][:, NB0:NB0 + 1304].astype(np.float64)
    nq, nkc, nvc, nks, nvs, nkw, nvw, ngate = np.split(proj, [512, 640, 768, 896, 1024, 1152, 1280], axis=-1)
    gelu = lambda z: 0.5 * z * (1 + erf(z / np.sqrt(2)))
    ncmp = S // 16 - 1
    tbl = inp["rel_bias"][:, :8].astype(np.float64)
    t = np.arange(S)
    outp = np.zeros((S, 512))
    gates = 1 / (1 + np.exp(-ngate.reshape(S, 8, 3)))
    for g in range(2):
        def comp(z, pe, w1, w2):
            zz = z[:, g * 64:(g + 1) * 64]
            blocks = np.stack([zz[16 * c:16 * c + 32] + pe for c in range(ncmp)])
            return gelu(blocks.reshape(ncmp, 2048) @ w1) @ w2
        kc = comp(nkc, inp["nsa_pe_k"][l], inp["nsa_ck_w1"][l], inp["nsa_ck_w2"][l])
        vc = comp(nvc, inp["nsa_pe_v"][l], inp["nsa_cv_w1"][l], inp["nsa_cv_w2"][l])
        cend = np.arange(ncmp) * 16 + 31
        vis = cend[None, :] <= t[:, None]
        pcs = []
        for hh in range(4):
            head = g * 4 + hh
            qh = nq[:, head * 64:(head + 1) * 64]
            sc = np.where(vis, qh @ kc.T * 0.125, -np.inf)
            m = sc.max(-1, keepdims=True); m = np.where(np.isfinite(m), m, 0.0)
            e = np.exp(sc - m); pcs.append(e / np.maximum(e.sum(-1, keepdims=True), 1e-30))
        nsel = 128
        ov = np.zeros((ncmp, nsel))
        for c in range(ncmp):
            for n in range(nsel):
                if 16 * c + 31 >= 64 * n and 16 * c <= 64 * n + 63:
                    ov[c, n] = 1
        imp = sum(pcs) @ ov
        cur = t // 64
        blk = np.arange(nsel)
        forced = (blk[None, :] == 0) | (blk[None, :] == cur[:, None]) | (blk[None, :] == cur[:, None] - 1)
        valid = blk[None, :] <= cur[:, None]
        imp = np.where(forced, 1e9, np.where(valid, imp, -1.0))
        order = np.argsort(-imp, axis=-1, kind="stable")[:, :16]
        selm = np.zeros((S, nsel), bool)
        np.put_along_axis(selm, order, True, axis=-1)
        keymask = np.repeat(selm, 64, axis=1)[:, :S]
        dist = t[:, None] - t[None, :]
        bk = t5_bucket_np(dist)
        for hh in range(4):
            head = g * 4 + hh
            qh = nq[:, head * 64:(head + 1) * 64]
            o_cmp = pcs[hh] @ vc
            ss = qh @ nks[:, g * 64:(g + 1) * 64].T * 0.125 + tbl[bk, head]
            ss = np.where(keymask & (dist >= 0), ss, -np.inf)
            e = np.exp(ss - ss.max(-1, keepdims=True)); ps = e / e.sum(-1, keepdims=True)
            o_sel = ps @ nvs[:, g * 64:(g + 1) * 64]
            sw = qh @ nkw[:, g * 64:(g + 1) * 64].T * 0.125 + tbl[bk, head]
            sw = np.where((dist >= 0) & (dist < 512), sw, -np.inf)
            e = np.exp(sw - sw.max(-1, keepdims=True)); pw = e / e.sum(-1, keepdims=True)
            o_win = pw @ nvw[:, g * 64:(g + 1) * 64]
            gg = gates[:, head]
            outp[:, head * 64:(head + 1) * 64] = gg[:, 0:1] * o_cmp + gg[:, 1:2] * o_sel + gg[:, 2:3] * o_win
    return outp


NE = 16384


def build_peer(T, final_norm=False):
    nc = bass.Bass("TRN2", target_bir_lowering=False)
    xT = nc.dram_tensor("xT", [D, T], F32, kind="ExternalInput").ap()
    xtm = nc.dram_tensor("xtm", [T, D], F32, kind="ExternalInput").ap()
    lnw = nc.dram_tensor("lnw", [128, 8], F32, kind="ExternalInput").ap()
    wq = nc.dram_tensor("wq", [128, 8, 2048], F32, kind="ExternalInput").ap()
    skT = nc.dram_tensor("skT", [128, 16, 128], F32, kind="ExternalInput").ap()
    uT = nc.dram_tensor("uT", [D, NE], F32, kind="ExternalInput").ap()
    vt = nc.dram_tensor("vt", [NE, D], F32, kind="ExternalInput").ap()
    ident = nc.dram_tensor("ident", [128, 128], F32, kind="ExternalInput").ap()
    out = nc.dram_tensor("out", [T, D], F32, kind="ExternalOutput").ap()
    zsc = nc.dram_tensor("zsc", [128, 8, T], F32, kind="Internal").ap()
    qsc = nc.dram_tensor("qsc", [128, 16, T], F32, kind="Internal").ap()
    nblk = T // 128
    with ExitStack() as stack:
        kb = KB(nc, stack)
        zsc_b = Buf(); qsc_b = Buf()
        pp = [kb.ps(f"pp{i}", [128, 512]) for i in range(2)]; pp_b = [Buf() for _ in range(2)]
        with ExitStack() as st1:
            kb.stack = st1
            npj = NormProj(kb, xT, lnw, T)
            stg = kb.sb("stg", [128, 2048], F32); stg_b = Buf()
            wq_b = kb.sb("wq_b", [128, 8, 2048], BF16); wq_bb = Buf()
            for c in range(8):
                kb.op("sp", lambda e, c=c: e.dma_start(out=stg[:], in_=wq[:, c, :]), writes=[stg_b], dma=True)
                kb.op("dve", lambda e, c=c: e.tensor_copy(out=wq_b[:, c, :], in_=stg[:]), reads=[stg_b], writes=[wq_bb])
            zf = kb.sb("zf", [128, 8, 512], F32); zf_b = Buf()
            qf = [kb.sb(f"qf{i}", [128, 512], F32) for i in range(2)]; qf_b = [Buf() for _ in range(2)]
            npj.load(0)
            for tt in range(npj.nt):
                if tt + 1 < npj.nt:
                    npj.load(tt + 1)
                u, u_b = npj.norm(tt)
                x = npj.x_sb[tt % 2]; x_b = npj.x_b[tt % 2]
                for c in range(8):
                    kb.op("dve", lambda e, c=c, x=x: e.scalar_tensor_tensor(out=zf[:, c, :], in0=x[:, c, :], scalar=npj.lnw32[:, c:c + 1],
                                                                          in1=npj.rstd_sb[:], op0=ALU.mult, op1=ALU.mult),
                          reads=[x_b, npj.rstd_b, npj.lnw32_b], writes=[zf_b] if c == 7 else [])
                kb.op("sp", lambda e, tt=tt: e.dma_start(out=zsc[:, :, tt * 512:(tt + 1) * 512], in_=zf[:]), reads=[zf_b], writes=[zsc_b], dma=True)
                for hp in range(16):
                    p = pp[hp % 2]
                    kb.op("pe", [lambda e, c=c, hp=hp, p=p, u=u: e.matmul(p[:], lhsT=wq_b[:, c, hp * 128:(hp + 1) * 128], rhs=u[:, c, :],
                                                                        start=(c == 0), stop=(c == 7)) for c in range(8)],
                          reads=[u_b, wq_bb], writes=[pp_b[hp % 2]])
                    kb.op("act", lambda e, hp=hp, p=p: e.activation(out=qf[hp % 2][:], in_=p[:], func=AF.Copy), reads=[pp_b[hp % 2]], writes=[qf_b[hp % 2]])
                    kb.op("sp", lambda e, hp=hp, tt=tt: e.dma_start(out=qsc[:, hp, tt * 512:(tt + 1) * 512], in_=qf[hp % 2][:]),
                          reads=[qf_b[hp % 2]], writes=[qsc_b], dma=True)
            kb_barrier(kb)
        kb.stack = stack
        sk_s = kb.sb("sk_s", [128, 16, 128], F32); sk_b = Buf()
        id_s = kb.sb("id_s", [128, 128], F32); id_b = Buf()
        kb.op("sp", lambda e: e.dma_start(out=sk_s[:], in_=skT), writes=[sk_b], dma=True)
        kb.op("sp", lambda e: e.dma_start(out=id_s[:], in_=ident), writes=[id_b], dma=True)
        G = kb.sb("G", [128, NE], BF16); G_b = Buf()
        zb = kb.sb("zb", [128, 8, 128], F32); zb_b = Buf()
        qb = kb.sb("qb", [128, 16, 128], F32); qb_b = Buf()
        s_tm = kb.sb("s_tm", [128, 16, 128], F32); s_b = Buf()
        ub = [kb.sb(f"ub{i}", [128, 8, 512], F32) for i in range(2)]; ub_b = [Buf() for _ in range(2)]
        vb = [kb.sb(f"vb{i}", [128, 4, 1024], F32) for i in range(2)]; vb_b = [Buf() for _ in range(2)]
        csum = kb.sb("csum", [128, 4096], F32); cs_b = Buf()
        wch = kb.sb("wch", [128, 4096], F32); wch_b = Buf()
        tmpg = kb.sb("tmpg", [128, 4096], BF16); tg_b = Buf()
        sc1 = kb.sb("sc1", [128, 128], F32); sc1_b = Buf()
        t12 = kb.sb("t12", [128, 2, 16], F32); t12_b = Buf()
        cand = kb.sb("cand", [128, 256], F32); cand_b = Buf()
        cand2 = kb.sb("cand2", [128, 256], F32); cand2_b = Buf()
        c16 = kb.sb("c16", [128, 16], F32); c16_b = Buf()
        ex16 = kb.sb("ex16", [128, 16], F32); ex16_b = Buf()
        stt = kb.sb("stt", [128, 8], F32); stt_b = Buf()
        gl = [kb.sb(f"gl{i}", [128, 512], F32) for i in range(2)]; gl_b = [Buf() for _ in range(2)]
        act = [kb.sb(f"act{i}", [128, 512], F32) for i in range(2)]; act_b = [Buf() for _ in range(2)]
        actT = [kb.sb(f"actT{i}", [128, 512], F32) for i in range(2)]; actT_b = [Buf() for _ in range(2)]
        xo = kb.sb("xo", [128, 1024], F32); xo_b = Buf()
        ps_s = pp[0]; ps_sb = pp_b[0]
        ph = [kb.ps(f"ph{i}", [128, 512]) for i in range(2)]; ph_b = [Buf() for _ in range(2)]
        ptr = kb.ps("ptr", [128, 512]); ptr_b = Buf()
        po = [kb.ps(f"po{i}", [128, 512]) for i in range(2)]; po_b = [Buf() for _ in range(2)]
        uTv = uT.rearrange("(c p) e -> p c e", p=128)
        vtv = vt.rearrange("(g k p) d -> g p k d", k=4, p=128)
        NEG_ = NE // 512

        def top16(src_ap, src_b, scratch, scratch_b, dst, dst_b):
            kb.op("dve", lambda e: e.max(out=dst[:, 0:8], in_=src_ap), reads=[src_b], writes=[dst_b])
            kb.op("dve", lambda e: e.match_replace(out=scratch, in_to_replace=dst[:, 0:8], in_values=src_ap, imm_value=-1e30),
                  reads=[src_b, dst_b], writes=[scratch_b])
            kb.op("dve", lambda e: e.max(out=dst[:, 8:16], in_=scratch), reads=[scratch_b], writes=[dst_b])

        for blk in range(nblk):
            ts = slice(blk * 128, (blk + 1) * 128)
            kb.op("sp", lambda e, ts=ts: e.dma_start(out=zb[:], in_=zsc[:, :, ts]), reads=[zsc_b], writes=[zb_b], dma=True)
            kb.op("sp", lambda e, ts=ts: e.dma_start(out=qb[:], in_=qsc[:, :, ts]), reads=[qsc_b], writes=[qb_b], dma=True)
            kb.op("sp", lambda e, ts=ts: e.dma_start(out=xo[:], in_=xtm[ts, :]), writes=[xo_b], dma=True)
            def load_eg(eg):
                i = eg % 2
                kb.op("sp", lambda e, eg=eg, i=i: e.dma_start(out=ub[i][:], in_=uTv[:, :, eg * 512:(eg + 1) * 512]), writes=[ub_b[i]], dma=True)
                kb.op("sp", lambda e, eg=eg, i=i: e.dma_start(out=vb[i][:], in_=vtv[eg]), writes=[vb_b[i]], dma=True)
            load_eg(0)
            for g4 in range(4):
                kb.op("pe", [lambda e, hp=hp: e.matmul(ps_s[:, (hp % 4) * 128:(hp % 4 + 1) * 128], lhsT=qb[:, hp, :], rhs=sk_s[:, hp, :], start=True, stop=True)
                             for hp in range(g4 * 4, g4 * 4 + 4)], reads=[qb_b, sk_b], writes=[ps_sb])
                kb.op("act", lambda e, g4=g4: e.activation(out=s_tm[:, g4 * 4:(g4 + 1) * 4, :], in_=ps_s[:].rearrange("p (a b) -> p a b", a=4), func=AF.Copy),
                      reads=[ps_sb], writes=[s_b])
            for h in range(8):
                s1 = s_tm[:, 2 * h, :]; s2 = s_tm[:, 2 * h + 1, :]
                top16(s1, s_b, sc1[:], sc1_b, t12[:, 0, :], t12_b)
                top16(s2, s_b, sc1[:], sc1_b, t12[:, 1, :], t12_b)
                cv = cand[:].rearrange("p (a b) -> p a b", a=16)
                kb.op("dve", lambda e, cv=cv: e.tensor_tensor(out=cv, in0=t12[:, 0, :].unsqueeze(2).broadcast_to([128, 16, 16]),
                                                            in1=t12[:, 1, :].unsqueeze(1).broadcast_to([128, 16, 16]), op=ALU.add),
                      reads=[t12_b], writes=[cand_b])
                top16(cand[:], cand_b, cand2[:], cand2_b, c16, c16_b)
                kb.op("dve", lambda e: e.tensor_scalar(out=stt[:, 0:1], in0=c16[:, 0:1], scalar1=-1.0, scalar2=None, op0=ALU.mult), reads=[c16_b], writes=[stt_b])
                kb.op("act", lambda e: e.activation(out=ex16[:], in_=c16[:], func=AF.Exp, bias=stt[:, 0:1], scale=1.0, accum_out=stt[:, 1:2]),
                      reads=[c16_b, stt_b], writes=[ex16_b, stt_b])
                kb.op("act", lambda e: e.activation(out=stt[:, 2:3], in_=stt[:, 1:2], func=AF.Ln), reads=[stt_b], writes=[stt_b])
                kb.op("dve", lambda e: e.tensor_tensor(out=stt[:, 3:4], in0=stt[:, 0:1], in1=stt[:, 2:3], op=ALU.subtract), reads=[stt_b], writes=[stt_b])
                for ch in range(4):
                    i0 = ch * 32
                    c3 = csum[:].rearrange("p (a b) -> p a b", a=32)
                    kb.op("dve", lambda e, c3=c3, s1=s1, s2=s2, i0=i0: e.tensor_tensor(out=c3, in0=s1[:, i0:i0 + 32].unsqueeze(2).broadcast_to([128, 32, 128]),
                                                                                     in1=s2.unsqueeze(1).broadcast_to([128, 32, 128]), op=ALU.add),
                          reads=[s_b], writes=[cs_b])
                    kb.op("act", lambda e: e.activation(out=wch[:], in_=csum[:], func=AF.Exp, bias=stt[:, 3:4], scale=1.0), reads=[cs_b, stt_b], writes=[wch_b])
                    gsl = G[:, ch * 4096:(ch + 1) * 4096]
                    if h == 0:
                        kb.op("dve", lambda e, gsl=gsl: e.scalar_tensor_tensor(out=gsl, in0=csum[:], scalar=c16[:, 15:16], in1=wch[:], op0=ALU.is_ge, op1=ALU.mult),
                              reads=[cs_b, c16_b, wch_b], writes=[G_b])
                    else:
                        kb.op("dve", lambda e: e.scalar_tensor_tensor(out=tmpg[:], in0=csum[:], scalar=c16[:, 15:16], in1=wch[:], op0=ALU.is_ge, op1=ALU.mult),
                              reads=[cs_b, c16_b, wch_b], writes=[tg_b])
                        kb.op("dve", lambda e, gsl=gsl: e.tensor_tensor(out=gsl, in0=gsl, in1=tmpg[:], op=ALU.add), reads=[tg_b, G_b], writes=[G_b])
            for eg in range(NEG_):
                if eg + 1 < NEG_:
                    load_eg(eg + 1)
                i = eg % 2
                kb.op("pe", [lambda e, c=c, i=i: e.matmul(ph[i][:], lhsT=zb[:, c, :], rhs=ub[i][:, c, :], start=(c == 0), stop=(c == 7)) for c in range(8)],
                      reads=[zb_b, ub_b[i]], writes=[ph_b[i]])
                kb.op("act", lambda e, i=i: e.activation(out=gl[i][:], in_=ph[i][:], func=AF.Gelu), reads=[ph_b[i]], writes=[gl_b[i]])
                kb.op("dve", lambda e, i=i, eg=eg: e.tensor_tensor(out=act[i][:], in0=gl[i][:], in1=G[:, eg * 512:(eg + 1) * 512], op=ALU.mult),
                      reads=[gl_b[i], G_b], writes=[act_b[i]])
                kb.op("pe", [lambda e, k=k, i=i: e.transpose(out=ptr[:, k * 128:(k + 1) * 128], in_=act[i][:, k * 128:(k + 1) * 128], identity=id_s[:]) for k in range(4)],
                      reads=[act_b[i], id_b], writes=[ptr_b])
                kb.op("act", lambda e, i=i: e.activation(out=actT[i][:], in_=ptr[:], func=AF.Copy), reads=[ptr_b], writes=[actT_b[i]])
                for hf in range(2):
                    kb.op("pe", [lambda e, k=k, i=i, hf=hf, eg=eg: e.matmul(po[hf][:], lhsT=actT[i][:, k * 128:(k + 1) * 128], rhs=vb[i][:, k, hf * 512:(hf + 1) * 512],
                                                                          start=(eg == 0 and k == 0), stop=(eg == NEG_ - 1 and k == 3)) for k in range(4)],
                          reads=[actT_b[i], vb_b[i]], writes=[po_b[hf]])
            for hf in range(2):
                kb.op("dve", lambda e, hf=hf: e.tensor_tensor(out=xo[:, hf * 512:(hf + 1) * 512], in0=xo[:, hf * 512:(hf + 1) * 512], in1=po[hf][:], op=ALU.add),
                      reads=[po_b[hf], xo_b], writes=[xo_b])
            kb.op("sp", lambda e, ts=ts: e.dma_start(out=out[ts, :], in_=xo[:]), reads=[xo_b], dma=True)
        kb.finish("sp")
        kb.emit()
    return nc


def peer_host_inputs(inp, l, xs):
    sk = inp["peer_subkeys"][l]
    return {
        "xT": np.ascontiguousarray(xs.T), "xtm": np.ascontiguousarray(xs),
        "lnw": np.ascontiguousarray(inp["ln2_w"][l].reshape(8, 128).T),
        "wq": np.ascontiguousarray(inp["peer_wq"][l].reshape(8, 128, 2048).transpose(1, 0, 2)),
        "skT": np.ascontiguousarray(sk.reshape(16, 128, 128).transpose(2, 0, 1)),
        "uT": np.ascontiguousarray(inp["peer_u"][l].T), "vt": np.ascontiguousarray(inp["peer_v"][l]),
        "ident": np.eye(128, dtype=np.float32),
    }


def peer_numpy_ref(inp, l, xs):
    from scipy.special import erf
    x = xs.astype(np.float64)
    z = x / np.sqrt((x * x).mean(-1, keepdims=True) + 1e-6) * inp["ln2_w"][l]
    q = (z @ inp["peer_wq"][l].astype(np.float64)).reshape(-1, 8, 2, 128)
    s = np.einsum('thpd,hpnd->thpn', q, inp["peer_subkeys"][l].astype(np.float64))
    T = x.shape[0]
    outp = np.zeros_like(x)
    gelu = lambda v: 0.5 * v * (1 + erf(v / np.sqrt(2)))
    for t in range(T):
        for h in range(8):
            i_top = np.argsort(-s[t, h], axis=-1, kind="stable")[:, :16]
            s_top = np.take_along_axis(s[t, h], i_top, axis=-1)
            cand = (s_top[0][:, None] + s_top[1][None, :]).reshape(-1)
            fl = np.argsort(-cand, kind="stable")[:16]
            sc = cand[fl]
            ex = i_top[0][fl // 16] * 128 + i_top[1][fl % 16]
            gate = np.exp(sc - sc.max()); gate /= gate.sum()
            hh = inp["peer_u"][l][ex].astype(np.float64) @ z[t]
            outp[t] += (gate * gelu(hh)) @ inp["peer_v"][l][ex].astype(np.float64)
    return x + outp, outp


_PROGS = {}


def _prog(name, fn, *a):
    key = (name,) + a
    if key not in _PROGS:
        _PROGS[key] = fn(*a)
    return _PROGS[key]


def _launch(name, nc, in_maps):
    t0 = time.time()
    res = run_bass_kernel_spmd(nc, in_maps, core_ids=list(range(8)))
    print(f"[kernel] launch {name}: {time.time() - t0:.1f}s", file=sys.stderr, flush=True)
    return res


def kernel(**inp):
    inp = {k: np.asarray(v) for k, v in inp.items()}
    B, S, T = 2, 8192, 2048
    x = np.ascontiguousarray(inp["x"], dtype=np.float32)
    cores = list(range(8))
    sl = lambda a, c: a[c // 4, (c % 4) * T:(c % 4 + 1) * T]
    for l in range(2):
        mix = []
        for name, build, host in (("rwkv", build_rwkv, rwkv_host_inputs), ("nsa", build_nsa, nsa_host_inputs), ("swa", build_swa, swa_host_inputs)):
            res = _launch(f"{name}{l}", _prog(name, build, S), [host(inp, l, c // 4, c % 4, S, x) for c in cores])
            o = np.empty((B, S, 512), np.float32)
            for c in cores:
                o[c // 4, :, (c % 4) * 128:(c % 4 + 1) * 128] = res.results[c]["out"]
            mix.append(o)
        o_a, o_b, o_c = mix
        res = _launch(f"merge{l}", _prog("merge", build_merge, T),
                      [merge_host_inputs(inp, l, sl(x, c), sl(o_a, c), sl(o_b, c), sl(o_c, c)) for c in cores])
        xm = np.empty_like(x)
        for c in cores:
            xm[c // 4, (c % 4) * T:(c % 4 + 1) * T] = res.results[c]["out"].T
        res = _launch(f"peer{l}", _prog("peer", build_peer, T), [peer_host_inputs(inp, l, sl(xm, c)) for c in cores])
        x = np.empty_like(xm)
        for c in cores:
            x[c // 4, (c % 4) * T:(c % 4 + 1) * T] = res.results[c]["out"]
    res = _launch("final", _prog("final", build_final, T), [{"xT": np.ascontiguousarray(sl(x, c).T),
                                                             "lnw": np.ascontiguousarray(inp["lnf_w"].reshape(8, 128).T)} for c in cores])
    out = np.empty_like(x)
    for c in cores:
        out[c // 4, (c % 4) * T:(c % 4 + 1) * T] = res.results[c]["out"].T
    return out
```
